# Optimizing a Trainium2 kernel written in Bass

```python
import math
import jax, jax.numpy as jnp
from jax import lax
import numpy as np

D_MODEL = 2048
BATCH = 8
SEQ = 2048
DEPTH = 1

MEM_LEN = 256
HEAD_DIM = 128
NSA_HEADS = 8
NSA_KV_HEADS = 2
NSA_GROUP = NSA_HEADS // NSA_KV_HEADS
NSA_WIDTH = NSA_HEADS * HEAD_DIM
NSA_KV_WIDTH = NSA_KV_HEADS * HEAD_DIM
CMP_LEN = 32
CMP_STRIDE = 16
CMP_HIDDEN = 2 * HEAD_DIM
SLC_LEN = 64
SLC_TOPK = 16
WINDOW = 512
SLC_QBLK = 32
WIN_QBLK = 128
POOL_WINDOWS = (2, 4, 8, 16)
POOL_GROUPS = 4
POOL_CH = 128
POOL_WIDTH = POOL_GROUPS * POOL_CH
MEM_HEADS = 4
MEM_HEAD_DIM = 128
MEM_WIDTH = MEM_HEADS * MEM_HEAD_DIM
MIX_WIDTH = NSA_WIDTH + POOL_WIDTH + MEM_WIDTH
IN_SPLITS = (NSA_WIDTH,
             NSA_KV_WIDTH, NSA_KV_WIDTH,
             NSA_KV_WIDTH, NSA_KV_WIDTH,
             NSA_KV_WIDTH, NSA_KV_WIDTH,
             3 * NSA_HEADS,
             POOL_WIDTH,
             MEM_WIDTH)
IN_WIDTH = sum(IN_SPLITS)
N_EXPERTS = 32
TOP_K = 4
D_EXPERT = 2048
SWIGLU_ALPHA = 1.702
SWIGLU_LIMIT = 7.0
MOE_BLK = 128
LN_EPS = 1e-5
DN_ALPHA = (2 * DEPTH) ** 0.25
DN_BETA = (8 * DEPTH) ** -0.25
NEG_INF = -1e30
BIG = 1e9

kernel_name = "hybrid_nsa_pool_mem_moe_deepnorm"


def _layer_norm(x, g, b):
    xf = x.astype(jnp.float32)
    mu = jnp.mean(xf, -1, keepdims=True)
    var = jnp.mean(jnp.square(xf - mu), -1, keepdims=True)
    return ((xf - mu) * lax.rsqrt(var + LN_EPS) * g + b).astype(x.dtype)


def _rms_norm(x, g):
    xf = x.astype(jnp.float32)
    y = xf * lax.rsqrt(jnp.mean(jnp.square(xf), -1, keepdims=True) + LN_EPS)
    return (y * g).astype(x.dtype)


def _masked_softmax(s, mask):
    s = jnp.where(mask, s.astype(jnp.float32), NEG_INF)
    return jax.nn.softmax(s, axis=-1) * mask


def _compress(kv, pos, w1, w2):
    B, S = kv.shape[:2]
    n_c = (S - CMP_LEN) // CMP_STRIDE + 1
    idx = jnp.arange(n_c)[:, None] * CMP_STRIDE + jnp.arange(CMP_LEN)[None, :]
    blk = kv[:, idx] + pos[None, None, :, None, :]
    blk = jnp.moveaxis(blk, 3, 2).reshape(B, n_c, NSA_KV_HEADS, CMP_LEN * HEAD_DIM)
    return jax.nn.gelu(blk @ w1) @ w2


def _nsa(q, k_c, v_c, k_s, v_s, k_w, v_w, gates,
         cmp_pos_k, cmp_w1_k, cmp_w2_k, cmp_pos_v, cmp_w1_v, cmp_w2_v):
    B, S = q.shape[:2]
    KVH, G, dh = NSA_KV_HEADS, NSA_GROUP, HEAD_DIM
    scale = dh ** -0.5
    q = q.reshape(B, S, KVH, G, dh)
    k_c, v_c, k_s, v_s, k_w, v_w = [a.reshape(B, S, KVH, dh) for a in (k_c, v_c, k_s, v_s, k_w, v_w)]
    t = jnp.arange(S)

    n_c = (S - CMP_LEN) // CMP_STRIDE + 1
    kc = _compress(k_c, cmp_pos_k, cmp_w1_k, cmp_w2_k)
    vc = _compress(v_c, cmp_pos_v, cmp_w1_v, cmp_w2_v)
    c_start = jnp.arange(n_c) * CMP_STRIDE
    mask_c = ((c_start + CMP_LEN - 1)[None, :] <= t[:, None])[None, :, None, None, :]
    s_c = jnp.einsum('bshgd,bchd->bshgc', q, kc) * scale
    p_c = _masked_softmax(s_c, mask_c)
    o_cmp = jnp.einsum('bshgc,bchd->bshgd', p_c.astype(vc.dtype), vc)

    n_s = S // SLC_LEN
    s_start = jnp.arange(n_s) * SLC_LEN
    overlap = jnp.clip(jnp.minimum((c_start + CMP_LEN)[:, None], (s_start + SLC_LEN)[None, :])
                       - jnp.maximum(c_start[:, None], s_start[None, :]), 0)
    overlap = (overlap // CMP_STRIDE).astype(jnp.float32)
    imp = jnp.einsum('bshc,cj->bshj', jnp.sum(p_c, axis=3), overlap)
    cur = t // SLC_LEN
    j = jnp.arange(n_s)
    valid = j[None, :] <= cur[:, None]
    forced = (j[None, :] == 0) | (j[None, :] == cur[:, None]) | (j[None, :] == cur[:, None] - 1)
    imp = jnp.where(forced[None, :, None, :], BIG,
                    jnp.where(valid[None, :, None, :], imp, -BIG))
    k_sel = min(SLC_TOPK, n_s)
    sel = lax.top_k(imp, k_sel)[1]

    k_blocks = k_s.reshape(B, n_s, SLC_LEN, KVH, dh).transpose(0, 3, 1, 2, 4)
    v_blocks = v_s.reshape(B, n_s, SLC_LEN, KVH, dh).transpose(0, 3, 1, 2, 4)
    nq = S // SLC_QBLK
    bi = jnp.arange(B)[:, None, None, None]
    hi = jnp.arange(KVH)[None, None, :, None]

    def slc_block(args):
        qb, selb, tb = args
        kb = k_blocks[bi, hi, selb]
        vb = v_blocks[bi, hi, selb]
        kpos = selb[..., None] * SLC_LEN + jnp.arange(SLC_LEN)
        mask = (kpos <= tb[None, :, None, None, None]).reshape(B, SLC_QBLK, KVH, 1, k_sel * SLC_LEN)
        s = jnp.einsum('bqhgd,bqhkld->bqhgkl', qb, kb) * scale
        p = _masked_softmax(s.reshape(B, SLC_QBLK, KVH, G, k_sel * SLC_LEN), mask)
        return jnp.einsum('bqhgn,bqhnd->bqhgd', p.astype(vb.dtype),
                          vb.reshape(B, SLC_QBLK, KVH, k_sel * SLC_LEN, dh))

    q_blk = q.reshape(B, nq, SLC_QBLK, KVH, G, dh).swapaxes(0, 1)
    sel_blk = sel.reshape(B, nq, SLC_QBLK, KVH, k_sel).swapaxes(0, 1)
    o_slc = lax.map(slc_block, (q_blk, sel_blk, t.reshape(nq, SLC_QBLK)))
    o_slc = o_slc.swapaxes(0, 1).reshape(B, S, KVH, G, dh)

    nb = S // WIN_QBLK
    span = WIN_QBLK + WINDOW
    kw_pad = jnp.pad(k_w, ((0, 0), (WINDOW, 0), (0, 0), (0, 0)))
    vw_pad = jnp.pad(v_w, ((0, 0), (WINDOW, 0), (0, 0), (0, 0)))

    def win_block(args):
        i, qb = args
        start = i * WIN_QBLK
        kb = lax.dynamic_slice_in_dim(kw_pad, start, span, axis=1)
        vb = lax.dynamic_slice_in_dim(vw_pad, start, span, axis=1)
        kpos = start - WINDOW + jnp.arange(span)
        tq = start + jnp.arange(WIN_QBLK)
        mask = ((kpos[None, :] <= tq[:, None]) & (kpos[None, :] > tq[:, None] - WINDOW)
                & (kpos[None, :] >= 0))[None, :, None, None, :]
        s = jnp.einsum('bqhgd,bkhd->bqhgk', qb, kb) * scale
        p = _masked_softmax(s, mask)
        return jnp.einsum('bqhgk,bkhd->bqhgd', p.astype(vb.dtype), vb)

    qw = q.reshape(B, nb, WIN_QBLK, KVH, G, dh).swapaxes(0, 1)
    o_win = lax.map(win_block, (jnp.arange(nb), qw)).swapaxes(0, 1).reshape(B, S, KVH, G, dh)

    g = jax.nn.sigmoid(gates.astype(jnp.float32)).reshape(B, S, KVH, G, 3).astype(q.dtype)
    o = g[..., 0:1] * o_cmp + g[..., 1:2] * o_slc + g[..., 2:3] * o_win
    return o.reshape(B, S, NSA_WIDTH)


def _pool_mixer(u, pool_w, pool_scale):
    B, S = u.shape[:2]
    uf = u.astype(jnp.float32).reshape(B, S, POOL_GROUPS, POOL_CH)
    cs = jnp.pad(jnp.cumsum(uf, axis=1), ((0, 0), (1, 0), (0, 0), (0, 0)))
    win = jnp.array(POOL_WINDOWS, dtype=jnp.int32)
    t = jnp.arange(S)
    lo = jnp.maximum(t[:, None] + 1 - win[None, :], 0)
    cnt = (t[:, None] + 1 - lo).astype(jnp.float32)
    gidx = jnp.arange(POOL_GROUPS)[None, :]
    mean = (cs[:, 1:] - cs[:, lo, gidx]) / cnt[None, :, :, None]
    pooled = (mean - uf).astype(u.dtype)
    y = jnp.einsum('bsgc,gcd->bsgd', pooled, pool_w) * pool_scale.reshape(POOL_GROUPS, POOL_CH)
    return y.reshape(B, S, POOL_WIDTH)


def _mem_attn(qm, mem, w_mem_kv):
    B, S = qm.shape[:2]
    M = mem.shape[1]
    km, vm = jnp.split(mem @ w_mem_kv, 2, axis=-1)
    qm = qm.reshape(B, S, MEM_HEADS, MEM_HEAD_DIM)
    km = km.reshape(B, M, MEM_HEADS, MEM_HEAD_DIM)
    vm = vm.reshape(B, M, MEM_HEADS, MEM_HEAD_DIM)
    s = jnp.einsum('bshd,bmhd->bhsm', qm, km) * (MEM_HEAD_DIM ** -0.5)
    p = jax.nn.softmax(s.astype(jnp.float32), axis=-1).astype(vm.dtype)
    return jnp.einsum('bhsm,bmhd->bshd', p, vm).reshape(B, S, MEM_WIDTH)


def _moe(h, w_router, b_router, w_gu, b_gu, w_dn, b_dn):
    B, S, D = h.shape
    T = B * S
    hf = h.reshape(T, D)
    logits = (hf @ w_router + b_router).astype(jnp.float32)
    top_val, top_idx = lax.top_k(logits, TOP_K)
    gate = jax.nn.softmax(top_val, axis=-1)
    A = T * TOP_K
    flat_e = top_idx.reshape(A)
    flat_tok = jnp.repeat(jnp.arange(T, dtype=jnp.int32), TOP_K)
    flat_g = gate.reshape(A)
    order = jnp.argsort(flat_e)
    se = flat_e[order]
    counts = jnp.bincount(flat_e, length=N_EXPERTS)
    starts = jnp.cumsum(counts) - counts
    pcounts = (counts + MOE_BLK - 1) // MOE_BLK * MOE_BLK
    pends = jnp.cumsum(pcounts)
    pstarts = pends - pcounts
    dest = pstarts[se] + (jnp.arange(A) - starts[se])
    n_pad = A + N_EXPERTS * MOE_BLK
    nblk = n_pad // MOE_BLK
    tok_buf = jnp.zeros((n_pad,), jnp.int32).at[dest].set(flat_tok[order])
    g_buf = jnp.zeros((n_pad,), jnp.float32).at[dest].set(flat_g[order])
    blk_e = jnp.minimum(jnp.searchsorted(pends, jnp.arange(nblk) * MOE_BLK, side='right'),
                        N_EXPERTS - 1)

    def expert_block(args):
        tok, g, e = args
        xb = hf[tok]
        gu = xb @ w_gu[e] + b_gu[e]
        x_glu = jnp.minimum(gu[:, ::2], SWIGLU_LIMIT)
        x_lin = jnp.clip(gu[:, 1::2], -SWIGLU_LIMIT, SWIGLU_LIMIT)
        act = x_glu * jax.nn.sigmoid(SWIGLU_ALPHA * x_glu) * (x_lin + 1)
        y = act @ w_dn[e] + b_dn[e]
        return y * g[:, None].astype(y.dtype)

    ys = lax.map(expert_block, (tok_buf.reshape(nblk, MOE_BLK), g_buf.reshape(nblk, MOE_BLK), blk_e))
    out = jnp.zeros((T, D), h.dtype).at[tok_buf].add(ys.reshape(n_pad, D))
    return out.reshape(B, S, D)


def _hybrid_layer(x, mem, w_in, cmp_pos_k, cmp_w1_k, cmp_w2_k, cmp_pos_v, cmp_w1_v, cmp_w2_v,
                  pool_w, pool_scale, w_mem_kv, gn_nsa, gn_pool, gn_mem, w_out, ln1_g, ln1_b,
                  w_router, b_router, w_gu, b_gu, w_dn, b_dn, ln2_g, ln2_b):
    proj = x @ w_in
    offs = [int(o) for o in np.cumsum(IN_SPLITS)[:-1]]
    q, k_c, v_c, k_s, v_s, k_w, v_w, gates, u_pool, q_mem = jnp.split(proj, offs, axis=-1)
    o_nsa = _nsa(q, k_c, v_c, k_s, v_s, k_w, v_w, gates,
                 cmp_pos_k, cmp_w1_k, cmp_w2_k, cmp_pos_v, cmp_w1_v, cmp_w2_v)
    o_pool = _pool_mixer(u_pool, pool_w, pool_scale)
    o_mem = _mem_attn(q_mem, mem, w_mem_kv)
    mixed = jnp.concatenate([_rms_norm(o_nsa, gn_nsa), _rms_norm(o_pool, gn_pool),
                             _rms_norm(o_mem, gn_mem)], axis=-1) @ w_out
    h = _layer_norm(DN_ALPHA * x + mixed, ln1_g, ln1_b)
    ffn = _moe(h, w_router, b_router, w_gu, b_gu, w_dn, b_dn)
    return _layer_norm(DN_ALPHA * h + ffn, ln2_g, ln2_b)


def setup_inputs(seed: int = 0) -> dict:
    key = jax.random.key(seed)
    ks = iter(jax.random.split(key, 32))
    L = DEPTH

    def nrm(shape, scale):
        return jax.random.normal(next(ks), shape, jnp.float32) * scale

    v_cols = (2, 4, 6)
    col_scale = jnp.concatenate([jnp.full((n,), DN_BETA if i in v_cols else 1.0, jnp.float32)
                                 for i, n in enumerate(IN_SPLITS)])
    mem_scale = jnp.concatenate([jnp.ones((MEM_WIDTH,), jnp.float32),
                                 jnp.full((MEM_WIDTH,), DN_BETA, jnp.float32)])
    cin = CMP_LEN * HEAD_DIM
    return {
        "x": nrm((BATCH, SEQ, D_MODEL), 1.0),
        "mem": nrm((BATCH, MEM_LEN, D_MODEL), 1.0),
        "w_in": nrm((L, D_MODEL, IN_WIDTH), D_MODEL ** -0.5) * col_scale,
        "cmp_pos_k": nrm((L, CMP_LEN, HEAD_DIM), 0.1),
        "cmp_w1_k": nrm((L, cin, CMP_HIDDEN), cin ** -0.5),
        "cmp_w2_k": nrm((L, CMP_HIDDEN, HEAD_DIM), CMP_HIDDEN ** -0.5),
        "cmp_pos_v": nrm((L, CMP_LEN, HEAD_DIM), 0.1),
        "cmp_w1_v": nrm((L, cin, CMP_HIDDEN), cin ** -0.5),
        "cmp_w2_v": nrm((L, CMP_HIDDEN, HEAD_DIM), CMP_HIDDEN ** -0.5),
        "pool_w": nrm((L, POOL_GROUPS, POOL_CH, POOL_CH), POOL_CH ** -0.5),
        "pool_scale": 1.0 + nrm((L, POOL_WIDTH), 0.1),
        "w_mem_kv": nrm((L, D_MODEL, 2 * MEM_WIDTH), D_MODEL ** -0.5) * mem_scale,
        "gn_nsa": 1.0 + nrm((L, NSA_WIDTH), 0.05),
        "gn_pool": 1.0 + nrm((L, POOL_WIDTH), 0.05),
        "gn_mem": 1.0 + nrm((L, MEM_WIDTH), 0.05),
        "w_out": nrm((L, MIX_WIDTH, D_MODEL), MIX_WIDTH ** -0.5) * DN_BETA,
        "ln1_g": 1.0 + nrm((L, D_MODEL), 0.05),
        "ln1_b": nrm((L, D_MODEL), 0.02),
        "w_router": nrm((L, D_MODEL, N_EXPERTS), D_MODEL ** -0.5),
        "b_router": nrm((L, N_EXPERTS), 0.01),
        "w_gu": nrm((L, N_EXPERTS, D_MODEL, 2 * D_EXPERT), D_MODEL ** -0.5) * DN_BETA,
        "b_gu": nrm((L, N_EXPERTS, 2 * D_EXPERT), 0.01),
        "w_dn": nrm((L, N_EXPERTS, D_EXPERT, D_MODEL), D_EXPERT ** -0.5) * DN_BETA,
        "b_dn": nrm((L, N_EXPERTS, D_MODEL), 0.01),
        "ln2_g": 1.0 + nrm((L, D_MODEL), 0.05),
        "ln2_b": nrm((L, D_MODEL), 0.02),
    }


def reference(x, mem, w_in, cmp_pos_k, cmp_w1_k, cmp_w2_k, cmp_pos_v, cmp_w1_v, cmp_w2_v,
              pool_w, pool_scale, w_mem_kv, gn_nsa, gn_pool, gn_mem, w_out, ln1_g, ln1_b,
              w_router, b_router, w_gu, b_gu, w_dn, b_dn, ln2_g, ln2_b):
    for l in range(DEPTH):
        x = _hybrid_layer(x, mem, w_in[l], cmp_pos_k[l], cmp_w1_k[l], cmp_w2_k[l],
                          cmp_pos_v[l], cmp_w1_v[l], cmp_w2_v[l], pool_w[l], pool_scale[l],
                          w_mem_kv[l], gn_nsa[l], gn_pool[l], gn_mem[l], w_out[l],
                          ln1_g[l], ln1_b[l], w_router[l], b_router[l], w_gu[l], b_gu[l],
                          w_dn[l], b_dn[l], ln2_g[l], ln2_b[l])
    return x
```

```python
import numpy as np
from contextlib import ExitStack
import concourse.bass as bass
import concourse.mybir as mybir
from concourse.bass_utils import run_bass_kernel_spmd

F32 = mybir.dt.float32
BF16 = mybir.dt.bfloat16
I32 = mybir.dt.int32
AF = mybir.ActivationFunctionType
ALU = mybir.AluOpType
AX = mybir.AxisListType

S_LEN = 2048
D = 2048
NT = 16
CAP = 1024
NCB = CAP // 128
NEXP = 32
NEG = -30000.0
SCALE = 128 ** -0.5
ALPHA = 2.0 ** 0.25
EPS = 1e-5


class _Op:
    __slots__ = ("eng", "fn", "deps", "is_dma", "sem", "val", "needs_inc", "idx", "ninst", "pred")


class Sched:
    ENGS = ("pe", "act", "dve", "pool", "sp")

    def __init__(self, nc, lanes=None):
        self.nc = nc
        self.ops = []
        self.last_write = {}
        self.readers = {}
        self.lanes = lanes or {"sp": 8, "act": 4, "pool": 6}
        self.dma_n = {q: 0 for q in self.lanes}
        self.lane_total = {q: [0] * n for q, n in self.lanes.items()}
        self.lane_last = {q: [None] * n for q, n in self.lanes.items()}
        self.esem = {}
        self.lsem = {}
        self.cnt = {e: 0 for e in self.ENGS}
        self.nemit = 0
        self.cnt_ap = None

    def alloc_sems(self, stack):
        nc = self.nc
        for e in ("pe", "act", "dve", "pool"):
            self.esem[e] = stack.enter_context(nc.semaphore("s_" + e))
        for q, n in self.lanes.items():
            self.lsem[q] = [stack.enter_context(nc.semaphore("l_%s%d" % (q, i))) for i in range(n)]

    def add(self, eng, fn, reads=(), writes=(), dma=False, ninst=1, pred=None):
        op = _Op()
        op.pred = pred
        op.eng = eng
        op.fn = fn
        op.is_dma = dma
        op.needs_inc = False
        op.idx = len(self.ops)
        op.ninst = ninst
        op.sem = None
        op.val = 0
        deps = set()
        for r in reads:
            lw = self.last_write.get(r)
            if lw is not None:
                deps.add(lw)
        for w in writes:
            lw = self.last_write.get(w)
            if lw is not None:
                deps.add(lw)
            for rd in self.readers.get(w, ()):
                deps.add(rd)
        for r in reads:
            self.readers.setdefault(r, []).append(op.idx)
        for w in writes:
            self.last_write[w] = op.idx
            self.readers[w] = []
        if dma:
            q = eng
            n = self.dma_n[q]
            self.dma_n[q] += 1
            lane = n % self.lanes[q]
            prev = self.lane_last[q][lane]
            if prev is not None:
                deps.add(prev)
            self.lane_total[q][lane] += 16 * ninst
            op.sem = ("lane", q, lane)
            op.val = self.lane_total[q][lane]
            self.lane_last[q][lane] = op.idx
        deps.discard(op.idx)
        op.deps = deps
        for d in deps:
            self.ops[d].needs_inc = True
        self.ops.append(op)
        return op.idx

    def emit(self):
        nc = self.nc
        cnt = self.cnt
        per = {e: [] for e in self.ENGS}
        for op in self.ops:
            per[op.eng].append(op)
        for e in ("pe", "act", "dve", "pool"):
            for op in reversed(per[e]):
                if not op.is_dma:
                    op.needs_inc = True
                    break
        for op in self.ops:
            if not op.is_dma and op.needs_inc:
                cnt[op.eng] += 1
                op.val = cnt[op.eng]
                op.sem = ("eng", op.eng)

        def semh(s):
            if s[0] == "eng":
                return self.esem[s[1]]
            return self.lsem[s[1]][s[2]]

        bname = {"pe": "tensor", "act": "scalar", "dve": "vector", "pool": "gpsimd", "sp": "sync"}
        ops = self.ops
        with nc.Block() as block:
            for eng in self.ENGS:

                def body(e, eng=eng):
                    seen = {}
                    st_ = {"reg": None, "ex": None}
                    for op in per[eng]:
                        for d in sorted(op.deps):
                            dop = ops[d]
                            if dop.eng == eng and eng == "pe" and not dop.is_dma:
                                continue
                            if seen.get(dop.sem, 0) >= dop.val:
                                continue
                            e.wait_ge(semh(dop.sem), dop.val)
                            seen[dop.sem] = dop.val
                        def run(op=op):
                            ins = op.fn(e)
                            if op.is_dma:
                                if not isinstance(ins, (list, tuple)):
                                    ins = [ins]
                                assert len(ins) == op.ninst, (len(ins), op.ninst)
                                for i_ in ins:
                                    i_.then_inc(semh(op.sem), 16)
                            elif op.needs_inc:
                                if isinstance(ins, (list, tuple)):
                                    ins = ins[-1]
                                ins.then_inc(semh(op.sem), 1)
                        if op.pred is None:
                            run()
                        else:
                            ex_, thr_, skip_fn = op.pred
                            if st_["reg"] is None:
                                st_["reg"] = e.alloc_register("pr_%s_%d" % (eng, self.nemit))
                            if st_["ex"] != ex_:
                                e.reg_load(st_["reg"], self.cnt_ap[0:1, ex_:ex_ + 1])
                                st_["ex"] = ex_
                            with e.If_lt(st_["reg"], thr_ + 1):
                                ins_ = skip_fn(e)
                                assert not op.is_dma
                                if op.needs_inc:
                                    ins_.then_inc(semh(op.sem), 1)
                            with e.Else():
                                run()
                    for q in self.lanes:
                        for lane in range(self.lanes[q]):
                            v = self.lane_total[q][lane]
                            if v > 0 and seen.get(("lane", q, lane), 0) < v:
                                e.wait_ge(self.lsem[q][lane], v)
                    for e2 in ("pe", "act", "dve", "pool"):
                        if e2 != eng and cnt[e2] > 0 and seen.get(("eng", e2), 0) < cnt[e2]:
                            e.wait_ge(self.esem[e2], cnt[e2])

                getattr(block, bname[eng])(body)
        self.ops = []
        self.last_write = {}
        self.readers = {}
        self.lane_last = {q: [None] * n for q, n in self.lanes.items()}
        self.nemit += 1


def make_consts():
    c = {}
    p = np.arange(128)
    c["c_ident"] = np.eye(128, dtype=np.float32)
    cb = np.where(p[:, None] <= p[None, :], 0.0, NEG).astype(np.float32)
    fb = np.where(p[:, None] > p[None, :], 0.0, NEG).astype(np.float32)
    c["c_cb"] = np.tile(cb, (1, 4))
    c["c_fb"] = np.tile(fb, (1, 4))
    cc = np.arange(128)[:, None, None]
    tt = np.arange(16)[None, :, None]
    tl = np.arange(128)[None, None, :]
    c["c_cm"] = ((16 * cc + 31 <= 128 * tt + tl) & (cc < 127)).astype(np.float32)
    cs = np.arange(128) * 16
    ss = np.arange(32) * 64
    ov = np.clip(np.minimum(cs[:, None] + 32, ss[None, :] + 64) - np.maximum(cs[:, None], ss[None, :]), 0, None) // 16
    ov[127, :] = 0
    aux = np.zeros((128, 33), np.float32)
    aux[:127, 0] = 1.0
    aux[:, 1:] = ov
    c["c_aux"] = aux
    j = np.arange(32)[:, None, None]
    kt = np.arange(16)[None, :, None]
    k = np.arange(128)[None, None, :]
    c["c_E"] = (j == 2 * kt + k // 64).astype(np.float32)
    t = (np.arange(16)[None, :, None] * 128 + np.arange(128)[:, None, None])
    jj = np.arange(32)[None, None, :]
    cur = t // 64
    valid = jj <= cur
    forced = (jj == 0) | (jj == cur) | (jj == cur - 1)
    c["c_m1"] = (valid & ~forced).astype(np.float32)
    c["c_m2"] = np.where(forced, 1e9, np.where(valid, 0.0, -1e9)).astype(np.float32)
    c["c_us"] = (p[:, None] < p[None, :]).astype(np.float32)
    c["c_ones"] = np.ones((128, 128), np.float32)
    c["c_ecap"] = np.tile((np.arange(32) * CAP).astype(np.float32)[None, :], (128, 1))
    corr = np.ones((128, 4, 16), np.float32)
    for g, w in enumerate((2, 4, 8, 16)):
        for t_ in range(16):
            corr[:, g, t_] = w / min(t_ + 1, w)
    c["c_corr"] = corr
    return c


CONST_SHAPES = {
    "c_ident": [128, 128], "c_cb": [128, 512], "c_fb": [128, 512], "c_cm": [128, 16, 128], "c_aux": [128, 33],
    "c_E": [32, 16, 128], "c_m1": [128, 16, 32], "c_m2": [128, 16, 32], "c_us": [128, 128], "c_ones": [128, 128],
    "c_ecap": [128, 32], "c_corr": [128, 4, 16],
}

IN_SHAPES = {
    "x": [2048, 2048], "mem": [256, 2048], "w_in": [2048, 3608],
    "cmp_pos_k": [32, 128], "cmp_w1_k": [4096, 256], "cmp_w2_k": [256, 128],
    "cmp_pos_v": [32, 128], "cmp_w1_v": [4096, 256], "cmp_w2_v": [256, 128],
    "pool_w": [4, 128, 128], "pool_scale": [1, 512], "w_mem_kv": [2048, 1024],
    "gn_nsa": [1, 1024], "gn_pool": [1, 512], "gn_mem": [1, 512], "w_out": [2048, 2048],
    "ln1_g": [1, 2048], "ln1_b": [1, 2048], "w_router": [2048, 32], "b_router": [1, 32],
    "w_gu": [32, 2048, 4096], "b_gu": [32, 4096], "w_dn": [32, 2048, 2048], "b_dn": [32, 2048],
    "ln2_g": [1, 2048], "ln2_b": [1, 2048],
}

BIGW = ("w_gu", "b_gu", "w_dn", "b_dn")


class K:
    def __init__(self, dbg=None):
        self.nsa_br = (0, 1, 2)
        if dbg in ("mixed_c", "mixed_s", "mixed_w"):
            self.nsa_br = ({"c": 0, "s": 1, "w": 2}[dbg[-1]],)
            dbg = "mixed"
        self.dbg = dbg
        nc = self.nc = bass.Bass("TRN2", target_bir_lowering=False)
        self.I = {}
        for n, shp in IN_SHAPES.items():
            if dbg is not None and n in BIGW:
                continue
            self.I[n] = nc.dram_tensor(n, shp, F32, kind="ExternalInput").ap()
        for n, shp in CONST_SHAPES.items():
            self.I[n] = nc.dram_tensor(n, shp, F32, kind="ExternalInput").ap()
        self.out = nc.dram_tensor("out", [2048, 2048], F32, kind="ExternalOutput").ap()
        mk = "ExternalOutput" if dbg in ("pm", "mixed") else "Internal"
        self.mixed = nc.dram_tensor("mixed", [2048, 2048], BF16, kind=mk).ap()
        self.hf = nc.dram_tensor("hf", [2048, 2048], F32, kind=("ExternalOutput" if dbg == "h" else "Internal")).ap()
        self.xbuf = nc.dram_tensor("xbuf", [NEXP * CAP, 2048], BF16, kind="Internal").ap()
        self.ysb = nc.dram_tensor("ysb", [NEXP * CAP, 2048], BF16, kind="Internal").ap()
        if dbg == "cmp":
            self.dkc = nc.dram_tensor("dkc", [128, 256], F32, kind="ExternalOutput").ap()
            self.dvc = nc.dram_tensor("dvc", [128, 322], F32, kind="ExternalOutput").ap()
        if dbg == "logits":
            self.dlog = nc.dram_tensor("dlog", [2048, 40], F32, kind="ExternalOutput").ap()

    def sb(self, st, name, shape, dt):
        return st.enter_context(self.nc.sbuf_tensor(name, shape, dt))

    def dma(self, q, out, in_, reads=(), writes=(), pred=None):
        self.S.add(q, lambda e: e.dma_start(out=out, in_=in_), reads=reads, writes=writes, dma=True, pred=pred)

    def mm_group(self, out, pairs, reads, writes, pred=None):
        def fn(e):
            last = None
            n = len(pairs)
            for i, (l, r) in enumerate(pairs):
                last = e.matmul(out, lhsT=l, rhs=r, start=(i == 0), stop=(i == n - 1))
            return last
        self.S.add("pe", fn, reads=reads, writes=writes, pred=pred)

    def build(self):
        nc = self.nc
        with ExitStack() as st:
            S = self.S = Sched(nc)
            S.alloc_sems(st)
            self._persist = st
            self.ps = [st.enter_context(nc.psum_tensor("ps%d" % i, [128, 512], F32)) for i in range(8)]
            self.load_consts(st)
            with ExitStack() as st1:
                self.phase_xT(st1)
                with ExitStack() as st2:
                    self.phase_pool(st2)
                with ExitStack() as st2:
                    self.phase_mem(st2)
                if self.dbg == "pm":
                    return nc
                with ExitStack() as stn:
                    self._nsa = stn
                    self.qT = self.sb(stn, "qT", [128, 8, 2048], BF16)
                    self.kcT = self.sb(stn, "kcT", [128, 2, 2048], BF16)
                    self.vcT = self.sb(stn, "vcT", [128, 2, 2048], BF16)
                    self.ksT = self.sb(stn, "ksT", [128, 2, 2048], BF16)
                    self.kwT = self.sb(stn, "kwT", [128, 2, 2048], BF16)
                    self.vs = self.sb(stn, "vs", [128, 16, 2, 129], BF16)
                    self.vw = self.sb(stn, "vw", [128, 16, 2, 129], BF16)
                    self.gsig = self.sb(stn, "gsig", [128, 16, 24], F32)
                    self.kccT = self.sb(stn, "kccT", [128, 2, 128], BF16)
                    self.vcc = self.sb(stn, "vcc", [128, 2, 161], BF16)
                    self.phase_qkv(stn)
                    with ExitStack() as st2:
                        self.phase_compress(st2)
                    if self.dbg == "cmp":
                        self.dma("pool", self.dkc, self.kccT[:].rearrange("p a b -> p (a b)"))
                        self.dma("pool", self.dvc, self.vcc[:].rearrange("p a b -> p (a b)"))
                        self.S.emit()
                        return nc
                    with ExitStack() as st2:
                        self.phase_nsa(st2)
            if self.dbg == "mixed":
                return nc
            with ExitStack() as st1:
                self.phase_wout(st1)
            if self.dbg in ("h", "logits"):
                return nc
            with ExitStack() as st1:
                self.phase_experts(st1)
            with ExitStack() as st1:
                self.phase_combine(st1)
        return nc

    def load_consts(self, st):
        S, I = self.S, self.I
        C = self.C = {}
        bf_list = ["c_ident", "c_cb", "c_fb", "c_cm", "c_aux", "c_E", "c_us", "c_ones"]
        for n in bf_list:
            C[n] = self.sb(st, "k" + n, CONST_SHAPES[n], BF16)
            self.dma("pool", C[n][:], I[n], writes=[n])
        for n in ["c_m1", "c_m2", "c_ecap", "c_corr"]:
            C[n] = self.sb(st, "k" + n, CONST_SHAPES[n], F32)
            self.dma("sp", C[n][:], I[n], writes=[n])
        C["ident_f"] = self.sb(st, "kidf", [128, 128], F32)
        self.dma("sp", C["ident_f"][:], I["c_ident"], writes=["ident_f"])
        S.emit()

    def transpose_rows(self, src_dram, ntile, dstT, st, tag):
        S, C = self.S, self.C
        xb = [self.sb(st, "xb%s%d" % (tag, i), [128, 2048], BF16) for i in range(2)]
        n = 0
        for tt in range(ntile):
            b = tt % 2
            self.dma("pool", xb[b][:], src_dram[tt * 128:(tt + 1) * 128, :], writes=["xb%d" % b])
            for grp in range(4):
                pb = n % 2
                n += 1
                psv = self.ps[pb][:].bitcast(BF16)

                def tr(e, b=b, grp=grp, psv=psv):
                    last = None
                    for i in range(4):
                        dk = grp * 4 + i
                        last = e.transpose(out=psv[:, i * 128:(i + 1) * 128], in_=xb[b][:, dk * 128:(dk + 1) * 128],
                                           identity=C["c_ident"][:])
                    return last
                S.add("pe", tr, reads=["xb%d" % b, "c_ident"], writes=["ps%d" % pb])
                dst = dstT[:, grp * 4:(grp + 1) * 4, tt * 128:(tt + 1) * 128]
                src = psv[:, 0:512].rearrange("p (a b) -> p a b", a=4)
                if n % 2 == 0:
                    S.add("act", lambda e, dst=dst, src=src: e.copy(out=dst, in_=src), reads=["ps%d" % pb], writes=[tag + "T"])
                else:
                    S.add("dve", lambda e, dst=dst, src=src: e.tensor_copy(out=dst, in_=src), reads=["ps%d" % pb], writes=[tag + "T"])

    def phase_xT(self, st):
        self.xT = self.sb(st, "xT", [128, 16, 2048], BF16)
        self.memT = self.sb(st, "memT", [128, 16, 256], BF16)
        with ExitStack() as st2:
            self.transpose_rows(self.I["x"], 16, self.xT, st2, "x")
            self.transpose_rows(self.I["mem"], 2, self.memT, st2, "mem")
            self.S.emit()

    def load_slab(self, dst, src_cols, name, q="pool"):
        self.dma(q, dst, src_cols.rearrange("(k p) n -> p k n", p=128), writes=[name])

    def proj_fm(self, wsl, wname, c0, dstT_ap_fn, dname, psbase=0, nps=4):
        S = self.S
        for tq in range(4):
            pb = psbase + (self._pn % nps)
            self._pn += 1
            pairs = [(wsl[:, dk, c0:c0 + 128], self.xT[:, dk, tq * 512:(tq + 1) * 512]) for dk in range(16)]
            self.mm_group(self.ps[pb][:], pairs, reads=[wname, "xT"], writes=["ps%d" % pb])
            dst = dstT_ap_fn(tq)
            if self._pn % 2 == 0:
                S.add("act", lambda e, dst=dst, pb=pb: e.copy(out=dst, in_=self.ps[pb][:]), reads=["ps%d" % pb], writes=[dname])
            else:
                S.add("dve", lambda e, dst=dst, pb=pb: e.tensor_copy(out=dst, in_=self.ps[pb][:]), reads=["ps%d" % pb], writes=[dname])

    def rmsnorm_store(self, src, width, gn_tile, gname, outbf, oname, dram_dst, st_tmp, key):
        S = self.S
        junk, ss, sq, rstd = st_tmp
        S.add("act", lambda e: e.activation(out=junk[:, 0:width], in_=src, func=AF.Square, accum_out=ss[:, 0:1]),
              reads=[key], writes=["rn_junk", "rn_ss"])
        S.add("act", lambda e: e.activation(out=sq[:, 0:1], in_=ss[:, 0:1], func=AF.Sqrt, scale=1.0 / width, bias=EPS),
              reads=["rn_ss"], writes=["rn_sq"])
        S.add("dve", lambda e: e.reciprocal(out=rstd[:, 0:1], in_=sq[:, 0:1]), reads=["rn_sq"], writes=["rn_rstd"])
        S.add("dve", lambda e: e.scalar_tensor_tensor(out=outbf, in0=src, scalar=rstd[:, 0:1], in1=gn_tile,
                                                      op0=ALU.mult, op1=ALU.mult),
              reads=[key, "rn_rstd", gname], writes=[oname])
        self.dma("sp", dram_dst, outbf, reads=[oname], writes=["mixed"])

    def rn_tmp(self, st, tag):
        junk = self.sb(st, "rnj" + tag, [128, 1024], F32)
        ss = self.sb(st, "rns" + tag, [128, 1], F32)
        sq = self.sb(st, "rnq" + tag, [128, 1], F32)
        rstd = self.sb(st, "rnr" + tag, [128, 1], F32)
        return (junk, ss, sq, rstd)

    def bcast_tile(self, st, name, src, width, q="sp"):
        t = self.sb(st, "bc_" + name, [128, width], F32)
        self.dma(q, t[:], src.partition_broadcast(128)[:, 0, :], writes=["bc_" + name])
        return t

    def phase_pool(self, st):
        S, I, C = self.S, self.I, self.C
        self._pn = 0
        wsl = self.sb(st, "wsl_p", [128, 16, 512], BF16)
        self.load_slab(wsl[:], I["w_in"][:, 2584:3096], "wsl_p")
        pw = self.sb(st, "pw", [128, 4, 128], BF16)
        self.dma("pool", pw[:], I["pool_w"].rearrange("g c d -> c g d"), writes=["pw"])
        psc = self.bcast_tile(st, "psc", I["pool_scale"], 512)
        gnp = self.bcast_tile(st, "gnp", I["gn_pool"], 512)
        PADW = 16
        up = self.sb(st, "up", [128, 4, PADW + 2048], F32)
        ta = self.sb(st, "pl_a", [128, PADW + 2048], F32)
        tb = self.sb(st, "pl_b", [128, PADW + 2048], F32)
        pooled = self.sb(st, "pooled", [128, 4, 2048], BF16)
        S.add("pool", lambda e: e.memset(up[:], 0.0), writes=["up"])
        S.add("pool", lambda e: e.memset(ta[:], 0.0), writes=["pl_a"])
        S.add("pool", lambda e: e.memset(tb[:], 0.0), writes=["pl_b"])
        for g in range(4):
            self.proj_fm(wsl, "wsl_p", g * 128, lambda tq, g=g: up[:, g, PADW + tq * 512:PADW + (tq + 1) * 512], "up")
        W = PADW + 2048
        for g, w in enumerate((2, 4, 8, 16)):
            src = up[:, g, :]
            sh = 1
            bufs = [ta, tb]
            bi = 0
            cur = src
            curname = "up"
            while sh < w:
                dst = bufs[bi]
                dname = "pl_a" if bi == 0 else "pl_b"
                S.add("dve", lambda e, dst=dst, cur=cur, sh=sh: e.tensor_tensor(out=dst[:, sh:W], in0=cur[:, sh:W], in1=cur[:, 0:W - sh], op=ALU.add),
                      reads=[curname], writes=[dname])
                cur = dst
                curname = dname
                bi ^= 1
                sh *= 2
            other = bufs[bi]
            oname = "pl_a" if bi == 0 else "pl_b"
            S.add("dve", lambda e, other=other, cur=cur, w=w: e.tensor_scalar(out=other[:, PADW:W], in0=cur[:, PADW:W], scalar1=1.0 / w, scalar2=None, op0=ALU.mult),
                  reads=[curname], writes=[oname])
            S.add("dve", lambda e, other=other, g=g: e.tensor_tensor(out=other[:, PADW:PADW + 16], in0=other[:, PADW:PADW + 16], in1=C["c_corr"][:, g, :], op=ALU.mult),
                  reads=[oname, "c_corr"], writes=[oname])
            S.add("dve", lambda e, other=other, g=g: e.tensor_tensor(out=pooled[:, g, :], in0=other[:, PADW:W], in1=up[:, g, PADW:W], op=ALU.subtract),
                  reads=[oname, "up"], writes=["pooled"])
        tmp = self.rn_tmp(st, "p")
        ot = [self.sb(st, "po%d" % i, [128, 512], F32) for i in range(2)]
        ob = [self.sb(st, "pob%d" % i, [128, 512], BF16) for i in range(2)]
        for tt in range(16):
            pb = 4 + tt % 2
            b = tt % 2

            def fn(e, tt=tt, pb=pb):
                last = None
                for g in range(4):
                    last = e.matmul(self.ps[pb][:, g * 128:(g + 1) * 128], lhsT=pooled[:, g, tt * 128:(tt + 1) * 128], rhs=pw[:, g, :], start=True, stop=True)
                return last
            S.add("pe", fn, reads=["pooled", "pw"], writes=["ps%d" % pb])
            S.add("dve", lambda e, b=b, pb=pb: e.tensor_tensor(out=ot[b][:], in0=self.ps[pb][:], in1=psc[:], op=ALU.mult),
                  reads=["ps%d" % pb, "bc_psc"], writes=["po%d" % b])
            self.rmsnorm_store(ot[b][:], 512, gnp[:], "bc_gnp", ob[b][:], "pob%d" % b,
                               self.mixed[tt * 128:(tt + 1) * 128, 1024:1536], tmp, "po%d" % b)
        S.emit()

    def phase_mem(self, st):
        S, I, C = self.S, self.I, self.C
        self._pn = 0
        wsl = self.sb(st, "wsl_m", [128, 16, 512], BF16)
        self.load_slab(wsl[:], I["w_in"][:, 3096:3608], "wsl_m")
        wk = self.sb(st, "wmk", [128, 16, 512], BF16)
        wv = self.sb(st, "wmv", [128, 16, 512], BF16)
        self.load_slab(wk[:], I["w_mem_kv"][:, 0:512], "wmk")
        self.load_slab(wv[:], I["w_mem_kv"][:, 512:1024], "wmv")
        gnm = self.bcast_tile(st, "gnm", I["gn_mem"], 512)
        qmT = self.sb(st, "qmT", [128, 4, 2048], BF16)
        kmT = self.sb(st, "kmT", [128, 4, 256], BF16)
        vm = self.sb(st, "vm", [128, 2, 512], BF16)
        for h in range(4):
            self.proj_fm(wsl, "wsl_m", h * 128, lambda tq, h=h: qmT[:, h, tq * 512:(tq + 1) * 512], "qmT")
        for h in range(4):
            pb = h % 2
            pairs = [(wk[:, dk, h * 128:(h + 1) * 128], self.memT[:, dk, :]) for dk in range(16)]
            self.mm_group(self.ps[pb][:, 0:256], pairs, reads=["wmk", "memT"], writes=["ps%d" % pb])
            S.add("act", lambda e, h=h, pb=pb: e.copy(out=kmT[:, h, :], in_=self.ps[pb][:, 0:256]), reads=["ps%d" % pb], writes=["kmT"])
        for mt in range(2):
            pb = 2 + mt
            pairs = [(self.memT[:, dk, mt * 128:(mt + 1) * 128], wv[:, dk, :]) for dk in range(16)]
            self.mm_group(self.ps[pb][:], pairs, reads=["wmv", "memT"], writes=["ps%d" % pb])
            S.add("dve", lambda e, mt=mt, pb=pb: e.tensor_copy(out=vm[:, mt, :], in_=self.ps[pb][:]), reads=["ps%d" % pb], writes=["vm"])
        pT = [self.sb(st, "mpT%d" % i, [128, 512], BF16) for i in range(4)]
        oacc = [self.sb(st, "moa%d" % i, [128, 4, 512], F32) for i in range(2)]
        den = self.sb(st, "mden", [128, 4], F32)
        rden = self.sb(st, "mrden", [128, 4], F32)
        tmp = self.rn_tmp(st, "m")
        ob = [self.sb(st, "mob%d" % i, [128, 512], BF16) for i in range(2)]
        n = 0
        for tq in range(4):
            oa = oacc[tq % 2]
            oan = "moa%d" % (tq % 2)
            for h in range(4):
                pts = []
                for mt in range(2):
                    pb = n % 2
                    pi = n % 4
                    n += 1
                    self.mm_group(self.ps[pb][:], [(kmT[:, h, mt * 128:(mt + 1) * 128], qmT[:, h, tq * 512:(tq + 1) * 512])],
                                  reads=["kmT", "qmT"], writes=["ps%d" % pb])
                    S.add("act", lambda e, pi=pi, pb=pb: e.activation(out=pT[pi][:], in_=self.ps[pb][:], func=AF.Exp, scale=SCALE),
                          reads=["ps%d" % pb], writes=["mpT%d" % pi])
                    pts.append(pi)
                ob_ = 4 + (tq * 4 + h) % 2
                db_ = 6

                def pv(e, pts=pts, h=h, ob_=ob_):
                    last = None
                    for tl in range(4):
                        for mt in range(2):
                            last = e.matmul(self.ps[ob_][:, tl * 128:(tl + 1) * 128], lhsT=pT[pts[mt]][:, tl * 128:(tl + 1) * 128],
                                            rhs=vm[:, mt, h * 128:(h + 1) * 128], start=(mt == 0), stop=(mt == 1))
                    for tl in range(4):
                        for mt in range(2):
                            last = e.matmul(self.ps[6][:, tl:tl + 1], lhsT=pT[pts[mt]][:, tl * 128:(tl + 1) * 128],
                                            rhs=C["c_ones"][:, 0:1], start=(mt == 0), stop=(mt == 1))
                    return last
                S.add("pe", pv, reads=["mpT%d" % pts[0], "mpT%d" % pts[1], "vm", "c_ones"], writes=["ps%d" % ob_, "ps6"])
                S.add("dve", lambda e: e.reciprocal(out=rden[:, 0:4], in_=self.ps[6][:, 0:4]), reads=["ps6"], writes=["mrden"])
                for tl in range(4):
                    S.add("dve", lambda e, tl=tl, h=h, oa=oa, ob_=ob_: e.tensor_scalar(
                        out=oa[:, tl, h * 128:(h + 1) * 128], in0=self.ps[ob_][:, tl * 128:(tl + 1) * 128],
                        scalar1=rden[:, tl:tl + 1], scalar2=None, op0=ALU.mult),
                        reads=["ps%d" % ob_, "mrden"], writes=[oan])
            for tl in range(4):
                tt = tq * 4 + tl
                b = tt % 2
                self.rmsnorm_store(oa[:, tl, :], 512, gnm[:], "bc_gnm", ob[b][:], "mob%d" % b,
                                   self.mixed[tt * 128:(tt + 1) * 128, 1536:2048], tmp, oan)
        S.emit()

    def phase_qkv(self, st):
        S, I = self.S, self.I
        self._pn = 0
        S.add("pool", lambda e: e.memset(self.vs[:, :, :, 128:129], 1.0), writes=["vs"])
        S.add("pool", lambda e: e.memset(self.vw[:, :, :, 128:129], 1.0), writes=["vw"])
        for h_ in range(2):
            S.add("dve", lambda e, h_=h_: e.tensor_copy(out=self.vcc[:, h_, 128:161], in_=self.C["c_aux"][:]), reads=["c_aux"], writes=["vcc"])
        with ExitStack() as st2:
            wsl = [self.sb(st2, "wsl_q%d" % i, [128, 16, 512], BF16) for i in range(2)]
            wg = self.sb(st2, "wsl_g", [128, 16, 24], BF16)
            self.load_slab(wg[:], I["w_in"][:, 2560:2584], "wsl_g")
            for si in range(5):
                b = si % 2
                wn = "wsl_q%d" % b
                self.load_slab(wsl[b][:], I["w_in"][:, si * 512:(si + 1) * 512], wn)
                if si < 2:
                    for hh in range(4):
                        self.proj_fm(wsl[b], wn, hh * 128, lambda tq, hd=si * 4 + hh: self.qT[:, hd, tq * 512:(tq + 1) * 512], "qT")
                elif si == 2:
                    for h in range(2):
                        self.proj_fm(wsl[b], wn, h * 128, lambda tq, h=h: self.kcT[:, h, tq * 512:(tq + 1) * 512], "kcT")
                        self.proj_fm(wsl[b], wn, 256 + h * 128, lambda tq, h=h: self.vcT[:, h, tq * 512:(tq + 1) * 512], "vcT")
                else:
                    kT = self.ksT if si == 3 else self.kwT
                    kn = "ksT" if si == 3 else "kwT"
                    vv = self.vs if si == 3 else self.vw
                    vn = "vs" if si == 3 else "vw"
                    for h in range(2):
                        self.proj_fm(wsl[b], wn, h * 128, lambda tq, h=h, kT=kT: kT[:, h, tq * 512:(tq + 1) * 512], kn)
                    for tt in range(16):
                        pb = 4 + tt % 2
                        pairs = [(self.xT[:, dk, tt * 128:(tt + 1) * 128], wsl[b][:, dk, 256:512]) for dk in range(16)]
                        self.mm_group(self.ps[pb][:, 0:256], pairs, reads=[wn, "xT"], writes=["ps%d" % pb])
                        S.add("act" if tt % 2 else "dve",
                              (lambda e, tt=tt, pb=pb, vv=vv: e.copy(out=vv[:, tt, :, 0:128], in_=self.ps[pb][:, 0:256].rearrange("p (a b) -> p a b", a=2))) if tt % 2 else
                              (lambda e, tt=tt, pb=pb, vv=vv: e.tensor_copy(out=vv[:, tt, :, 0:128], in_=self.ps[pb][:, 0:256].rearrange("p (a b) -> p a b", a=2))),
                              reads=["ps%d" % pb], writes=[vn])
            for tt in range(16):
                pb = 6 + tt % 2
                pairs = [(self.xT[:, dk, tt * 128:(tt + 1) * 128], wg[:, dk, :]) for dk in range(16)]
                self.mm_group(self.ps[pb][:, 0:24], pairs, reads=["wsl_g", "xT"], writes=["ps%d" % pb])
                S.add("act", lambda e, tt=tt, pb=pb: e.activation(out=self.gsig[:, tt, :], in_=self.ps[pb][:, 0:24], func=AF.Sigmoid),
                      reads=["ps%d" % pb], writes=["gsig"])
            S.emit()

    def phase_compress(self, st_unused):
        S, I, C = self.S, self.I, self.C
        for kind in range(2):
            with ExitStack() as st2:
                sfx = "k" if kind == 0 else "v"
                src = self.kcT if kind == 0 else self.vcT
                srcn = "kcT" if kind == 0 else "vcT"
                w1 = self.sb(st2, "cw1" + sfx, [128, 32, 256], BF16)
                self.dma("pool", w1[:], I["cmp_w1_" + sfx].rearrange("(l d) n -> d l n", d=128), writes=["cw1" + sfx])
                w2 = self.sb(st2, "cw2" + sfx, [128, 2, 128], BF16)
                self.dma("pool", w2[:], I["cmp_w2_" + sfx].rearrange("(a p) d -> p a d", p=128), writes=["cw2" + sfx])
                pos = self.sb(st2, "cpos" + sfx, [32, 128], F32)
                self.dma("sp", pos[:], I["cmp_pos_" + sfx], writes=["cpos" + sfx])
                posT = self.sb(st2, "cposT" + sfx, [128, 32], BF16)
                S.add("pe", lambda e, pos=pos: e.transpose(out=self.ps[7][:, 0:32], in_=pos[:], identity=C["ident_f"][0:32, 0:32]),
                      reads=["cpos" + sfx, "ident_f"], writes=["ps7"])
                S.add("dve", lambda e, posT=posT: e.tensor_copy(out=posT[:], in_=self.ps[7][:, 0:32]), reads=["ps7"], writes=["cposT" + sfx])
                bias = self.sb(st2, "cbias" + sfx, [128, 2], F32)
                for half in range(2):
                    pairs = [(w1[:, l, half * 128:(half + 1) * 128], posT[:, l:l + 1]) for l in range(32)]
                    self.mm_group(self.ps[6][:, half:half + 1], pairs, reads=["cw1" + sfx, "cposT" + sfx], writes=["ps6"])
                S.add("dve", lambda e, bias=bias: e.tensor_copy(out=bias[:], in_=self.ps[6][:, 0:2]), reads=["ps6"], writes=["cbias" + sfx])
                xs = self.sb(st2, "cxs" + sfx, [128, 128], F32)
                x2 = self.sb(st2, "cx2" + sfx, [128, 128], F32)
                sg = self.sb(st2, "csg" + sfx, [128, 128], F32)
                gT = [self.sb(st2, "cg%s%d" % (sfx, i), [128, 128], BF16) for i in range(2)]
                for h in range(2):
                    for half in range(2):
                        pb = half
                        pairs = [(w1[:, l, half * 128:(half + 1) * 128], src[:, h, l:l + 16 * 126 + 1:16]) for l in range(32)]
                        self.mm_group(self.ps[pb][:, 0:127], pairs, reads=["cw1" + sfx, srcn], writes=["ps%d" % pb])
                        S.add("act", lambda e, pb=pb, half=half, xs=xs, bias=bias: e.activation(out=xs[:, 0:127], in_=self.ps[pb][:, 0:127], func=AF.Identity,
                                                                             bias=bias[:, half:half + 1]),
                              reads=["ps%d" % pb, "cbias" + sfx], writes=["cxs"])
                        S.add("dve", lambda e, xs=xs, x2=x2: e.tensor_tensor(out=x2[:, 0:127], in0=xs[:, 0:127], in1=xs[:, 0:127], op=ALU.mult),
                              reads=["cxs"], writes=["cx2"])
                        S.add("dve", lambda e, x2=x2: e.tensor_scalar(out=x2[:, 0:127], in0=x2[:, 0:127], scalar1=0.044715, scalar2=1.0, op0=ALU.mult, op1=ALU.add),
                              reads=["cx2"], writes=["cx2"])
                        S.add("dve", lambda e, xs=xs, x2=x2: e.tensor_tensor(out=x2[:, 0:127], in0=x2[:, 0:127], in1=xs[:, 0:127], op=ALU.mult),
                              reads=["cx2", "cxs"], writes=["cx2"])
                        S.add("act", lambda e, x2=x2, sg=sg: e.activation(out=sg[:, 0:127], in_=x2[:, 0:127], func=AF.Sigmoid, scale=1.5957691216057308),
                              reads=["cx2"], writes=["csg"])
                        S.add("dve", lambda e, xs=xs, sg=sg, half=half, gT=gT: e.tensor_tensor(out=gT[half][:, 0:127], in0=xs[:, 0:127], in1=sg[:, 0:127], op=ALU.mult),
                              reads=["cxs", "csg"], writes=["cg%d" % half])
                    if kind == 0:
                        pairs = [(w2[:, half, :], gT[half][:, 0:127]) for half in range(2)]
                        self.mm_group(self.ps[2][:, 0:127], pairs, reads=["cw2" + sfx, "cg0", "cg1"], writes=["ps2"])
                        S.add("act", lambda e, h=h: e.copy(out=self.kccT[:, h, 0:127], in_=self.ps[2][:, 0:127]), reads=["ps2"], writes=["kccT"])
                    else:
                        pairs = [(gT[half][:, 0:127], w2[:, half, :]) for half in range(2)]
                        self.mm_group(self.ps[2][0:127, 0:128], pairs, reads=["cw2" + sfx, "cg0", "cg1"], writes=["ps2"])
                        S.add("act", lambda e, h=h: e.copy(out=self.vcc[0:127, h, 0:128], in_=self.ps[2][0:127, 0:128]), reads=["ps2"], writes=["vcc"])
                S.emit()

    def phase_nsa(self, st):
        S, I, C = self.S, self.I, self.C
        ps = self.ps
        gnn = self.bcast_tile(st, "gnn", I["gn_nsa"], 1024)
        ec = [self.sb(st, "ec%d" % i, [128, 512], BF16) for i in range(2)]
        pT = [self.sb(st, "npT%d" % i, [128, 512], BF16) for i in range(3)]
        oacc = [self.sb(st, "noa%d" % i, [128, 8, 128], F32) for i in range(2)]
        ob = [self.sb(st, "nob%d" % i, [128, 1024], BF16) for i in range(2)]
        rc = self.sb(st, "nrc", [128, 4], F32)
        coef = self.sb(st, "ncoef", [128, 4], F32)
        impg = self.sb(st, "nimpg", [128, 4, 32], F32)
        imp = self.sb(st, "nimp", [128, 32], F32)
        cmpm = self.sb(st, "ncmpm", [128, 32, 32], F32)
        rk = self.sb(st, "nrk", [128, 32], F32)
        nsel = self.sb(st, "nsel", [128, 32], BF16)
        nselT = [self.sb(st, "nselT%d" % i, [32, 128], BF16) for i in range(2)]
        tmp = self.rn_tmp(st, "n")
        nst = 0
        npt = 0
        nsl = 0
        for tt in range(16):
            t0 = tt * 128
            oa = oacc[tt % 2]
            oan = "noa%d" % (tt % 2)
            for h in range(2):
                Q = self.qT[:, 4 * h:4 * h + 4, t0:t0 + 128]
                ncv = min(127, 8 * tt + 7)
                pb = nst % 3
                nst += 1
                self.mm_group(ps[pb][0:ncv, :], [(self.kccT[:, h, 0:ncv], Q)], reads=["kccT", "qT"], writes=["ps%d" % pb])
                eb = (tt * 2 + h) % 2
                S.add("act", lambda e, eb=eb, pb=pb, ncv=ncv: e.activation(out=ec[eb][0:ncv, :], in_=ps[pb][0:ncv, :], func=AF.Exp, scale=SCALE),
                      reads=["ps%d" % pb], writes=["ec%d" % eb])
                S.add("pool", lambda e, eb=eb, ncv=ncv, tt=tt: e.tensor_tensor(
                    out=ec[eb][0:ncv, :].rearrange("p (a b) -> p a b", a=4), in0=ec[eb][0:ncv, :].rearrange("p (a b) -> p a b", a=4),
                    in1=C["c_cm"][0:ncv, tt:tt + 1, :].broadcast_to([ncv, 4, 128]), op=ALU.mult),
                    reads=["ec%d" % eb, "c_cm"], writes=["ec%d" % eb])

                def pvc(e, eb=eb, ncv=ncv, h=h):
                    last = None
                    for g in range(4):
                        last = e.matmul(ps[3 + g][:, 0:161], lhsT=ec[eb][0:ncv, g * 128:(g + 1) * 128], rhs=self.vcc[0:ncv, h, :], start=True, stop=True)
                    return last
                S.add("pe", pvc, reads=["ec%d" % eb, "vcc"], writes=["ps3", "ps4", "ps5", "ps6"])
                for g in range(4):
                    S.add("dve", lambda e, g=g: e.tensor_scalar(out=rc[:, g:g + 1], in0=ps[3 + g][:, 128:129], scalar1=1e-30, scalar2=None, op0=ALU.max),
                          reads=["ps%d" % (3 + g)], writes=["nrc"])
                S.add("dve", lambda e: e.reciprocal(out=rc[:, 0:4], in_=rc[:, 0:4]), reads=["nrc"], writes=["nrc"])
                S.add("dve", lambda e, tt=tt, h=h: e.tensor_tensor(out=coef[:, 0:4], in0=rc[:, 0:4], in1=self.gsig[:, tt, 12 * h + 0:12 * h + 12:3], op=ALU.mult),
                      reads=["nrc", "gsig"], writes=["ncoef"])
                first_acc = True
                if 0 in self.nsa_br:
                    first_acc = False
                    for g in range(4):
                        S.add("dve", lambda e, g=g, h=h, oa=oa: e.tensor_scalar(out=oa[:, 4 * h + g, :], in0=ps[3 + g][:, 0:128],
                                                                                 scalar1=coef[:, g:g + 1], scalar2=None, op0=ALU.mult),
                              reads=["ps%d" % (3 + g), "ncoef"], writes=[oan])
                use_sel = tt >= 8
                sb_ = None
                if use_sel:
                    for g in range(4):
                        S.add("dve", lambda e, g=g: e.tensor_scalar(out=impg[:, g, :], in0=ps[3 + g][:, 129:161], scalar1=rc[:, g:g + 1], scalar2=None, op0=ALU.mult),
                              reads=["ps%d" % (3 + g), "nrc"], writes=["nimpg"])
                    S.add("dve", lambda e: e.tensor_reduce(out=imp[:], in_=impg[:].rearrange("p g j -> p j g"), axis=AX.X, op=ALU.add),
                          reads=["nimpg"], writes=["nimp"])
                    S.add("dve", lambda e, tt=tt: e.tensor_tensor(out=imp[:], in0=imp[:], in1=C["c_m1"][:, tt, :], op=ALU.mult), reads=["nimp", "c_m1"], writes=["nimp"])
                    S.add("dve", lambda e, tt=tt: e.tensor_tensor(out=imp[:], in0=imp[:], in1=C["c_m2"][:, tt, :], op=ALU.add), reads=["nimp", "c_m2"], writes=["nimp"])
                    S.add("dve", lambda e: e.tensor_tensor(out=cmpm[:], in0=imp[:].unsqueeze(1).broadcast_to([128, 32, 32]),
                                                           in1=imp[:].unsqueeze(2).broadcast_to([128, 32, 32]), op=ALU.is_gt),
                          reads=["nimp"], writes=["ncmpm"])
                    S.add("dve", lambda e: e.tensor_reduce(out=rk[:], in_=cmpm[:], axis=AX.X, op=ALU.add), reads=["ncmpm"], writes=["nrk"])
                    S.add("dve", lambda e: e.tensor_scalar(out=nsel[:], in0=rk[:], scalar1=15.5, scalar2=NEG, op0=ALU.is_gt, op1=ALU.mult),
                          reads=["nrk"], writes=["nsel"])
                    sb_ = nsl % 2
                    nsl += 1
                    ps7b = ps[7][:].bitcast(BF16)
                    S.add("pe", lambda e, ps7b=ps7b: e.transpose(out=ps7b[0:32, 0:128], in_=nsel[:], identity=C["c_ident"][:]),
                          reads=["nsel", "c_ident"], writes=["ps7"])
                    S.add("act", lambda e, sb_=sb_, ps7b=ps7b: e.copy(out=nselT[sb_][:], in_=ps7b[0:32, 0:128]), reads=["ps7"], writes=["nselT%d" % sb_])
                for br in (1, 2):
                    kT = self.ksT if br == 1 else self.kwT
                    kn = "ksT" if br == 1 else "kwT"
                    vv = self.vs if br == 1 else self.vw
                    vn = "vs" if br == 1 else "vw"
                    obank = 4 if br == 1 else 5
                    dcol = 132 if br == 1 else 136
                    dname = "ps6s" if br == 1 else "ps6w"
                    kts = list(range(0, tt + 1)) if br == 1 else list(range(max(0, tt - 4), tt + 1))
                    for ki, kt in enumerate(kts):
                        pb = nst % 3
                        nst += 1
                        extra = []
                        rds = [kn, "qT"]
                        if br == 1 and use_sel:
                            rds += ["c_E", "nselT%d" % sb_]
                        if kt == tt:
                            rds += ["c_ident", "c_cb"]
                        if br == 2 and kt == tt - 4:
                            rds += ["c_ident", "c_fb"]

                        def sc(e, pb=pb, kT=kT, h=h, kt=kt, Q=Q, br=br, sb_=sb_, tt=tt):
                            mms = [(ps[pb][:], kT[:, h, kt * 128:(kt + 1) * 128], Q)]
                            if br == 1 and tt >= 8:
                                for g in range(4):
                                    mms.append((ps[pb][:, g * 128:(g + 1) * 128], C["c_E"][:, kt, :], nselT[sb_][:]))
                            if kt == tt:
                                mms.append((ps[pb][:], C["c_ident"][:], C["c_cb"][:]))
                            if br == 2 and kt == tt - 4:
                                mms.append((ps[pb][:], C["c_ident"][:], C["c_fb"][:]))
                            last = None
                            for i, (o, l, r) in enumerate(mms):
                                last = e.matmul(o, lhsT=l, rhs=r, start=(i == 0), stop=(i == len(mms) - 1))
                            return last
                        S.add("pe", sc, reads=rds, writes=["ps%d" % pb])
                        pi = npt % 3
                        npt += 1
                        S.add("act", lambda e, pi=pi, pb=pb: e.activation(out=pT[pi][:], in_=ps[pb][:], func=AF.Exp, scale=SCALE),
                              reads=["ps%d" % pb], writes=["npT%d" % pi])

                        def pv(e, pi=pi, vv=vv, kt=kt, h=h, first=(ki == 0), lastk=(ki == len(kts) - 1)):
                            last = None
                            for g in range(4):
                                last = e.matmul(ps[3 + g][:, 0:129], lhsT=pT[pi][:, g * 128:(g + 1) * 128],
                                                rhs=vv[:, kt, h, :], start=first, stop=lastk)
                            return last
                        S.add("pe", pv, reads=["npT%d" % pi, vn], writes=["ps3", "ps4", "ps5", "ps6"])
                    for g in range(4):
                        S.add("dve", lambda e, g=g: e.tensor_copy(out=rc[:, g:g + 1], in_=ps[3 + g][:, 128:129]), reads=["ps%d" % (3 + g)], writes=["nrc"])
                    S.add("dve", lambda e: e.reciprocal(out=rc[:, 0:4], in_=rc[:, 0:4]), reads=["nrc"], writes=["nrc"])
                    S.add("dve", lambda e, tt=tt, h=h, br=br: e.tensor_tensor(out=coef[:, 0:4], in0=rc[:, 0:4], in1=self.gsig[:, tt, 12 * h + br:12 * h + 12:3], op=ALU.mult),
                          reads=["nrc", "gsig"], writes=["ncoef"])
                    if br not in self.nsa_br:
                        continue
                    for g in range(4):
                        if first_acc:
                            S.add("dve", lambda e, g=g, h=h, oa=oa: e.tensor_scalar(out=oa[:, 4 * h + g, :], in0=ps[3 + g][:, 0:128],
                                                                                      scalar1=coef[:, g:g + 1], scalar2=None, op0=ALU.mult),
                                  reads=["ps%d" % (3 + g), "ncoef"], writes=[oan])
                        else:
                            S.add("dve", lambda e, g=g, h=h, oa=oa: e.scalar_tensor_tensor(
                                out=oa[:, 4 * h + g, :], in0=ps[3 + g][:, 0:128], scalar=coef[:, g:g + 1], in1=oa[:, 4 * h + g, :],
                                op0=ALU.mult, op1=ALU.add),
                                reads=["ps%d" % (3 + g), "ncoef", oan], writes=[oan])
                    first_acc = False
            b = tt % 2
            self.rmsnorm_store(oa[:].rearrange("p a b -> p (a b)"), 1024, gnn[:], "bc_gnn", ob[b][:], "nob%d" % b,
                               self.mixed[t0:t0 + 128, 0:1024], tmp, oan)
        S.emit()

    def layernorm(self, src, skey, dst, dkey, g_t, gname, b_t, bname, tmp):
        S = self.S
        stats, mv, sq, rstd = tmp
        for c in range(4):
            S.add("dve", lambda e, c=c: e.bn_stats(out=stats[:, c, :], in_=src[:, c * 512:(c + 1) * 512]), reads=[skey], writes=["ln_stats"])
        S.add("dve", lambda e: e.bn_aggr(out=mv[:, 0:2], in_=stats[:].rearrange("p a b -> p (a b)")), reads=["ln_stats"], writes=["ln_mv"])
        S.add("act", lambda e: e.activation(out=sq[:, 0:1], in_=mv[:, 1:2], func=AF.Sqrt, bias=EPS), reads=["ln_mv"], writes=["ln_sq"])
        S.add("dve", lambda e: e.reciprocal(out=rstd[:, 0:1], in_=sq[:, 0:1]), reads=["ln_sq"], writes=["ln_rstd"])
        S.add("dve", lambda e: e.tensor_scalar(out=dst, in0=src, scalar1=mv[:, 0:1], scalar2=rstd[:, 0:1], op0=ALU.subtract, op1=ALU.mult),
              reads=[skey, "ln_mv", "ln_rstd"], writes=[dkey])
        S.add("pool", lambda e: e.tensor_tensor(out=dst, in0=dst, in1=g_t[:], op=ALU.mult), reads=[dkey, gname], writes=[dkey])
        S.add("pool", lambda e: e.tensor_tensor(out=dst, in0=dst, in1=b_t[:], op=ALU.add), reads=[dkey, bname], writes=[dkey])

    def ln_tmp(self, st, tag):
        return (self.sb(st, "lns" + tag, [128, 4, 6], F32), self.sb(st, "lnm" + tag, [128, 2], F32),
                self.sb(st, "lnq" + tag, [128, 1], F32), self.sb(st, "lnr" + tag, [128, 1], F32))

    def phase_wout(self, st):
        S, I, C = self.S, self.I, self.C
        ps = self.ps
        P_ = self._persist
        self.slots = self.sb(P_, "slots", [128, 16, 4], I32)
        self.gates = self.sb(P_, "gatesk", [128, 16, 4], F32)
        self.cnti = self.sb(P_, "cnti", [128, 32], I32)
        S.cnt_ap = self.cnti
        wo = self.sb(st, "wo", [128, 16, 2048], BF16)
        for cg in range(4):
            self.dma("pool", wo[:, :, cg * 512:(cg + 1) * 512], I["w_out"][:, cg * 512:(cg + 1) * 512].rearrange("(k p) n -> p k n", p=128), writes=["wo"])
        wr = self.sb(st, "wr", [128, 16, 32], F32)
        self.dma("sp", wr[:], I["w_router"].rearrange("(k p) n -> p k n", p=128), writes=["wr"])
        g1 = self.bcast_tile(st, "g1", I["ln1_g"], 2048)
        b1 = self.bcast_tile(st, "b1", I["ln1_b"], 2048)
        br = self.bcast_tile(st, "br", I["b_router"], 32)
        mx = [self.sb(st, "mx%d" % i, [128, 2048], BF16) for i in range(2)]
        mxT = [self.sb(st, "mxT%d" % i, [128, 16, 128], BF16) for i in range(2)]
        xt = [self.sb(st, "xres%d" % i, [128, 2048], F32) for i in range(2)]
        pre = self.sb(st, "pre", [128, 2048], F32)
        hh = [self.sb(st, "hh%d" % i, [128, 2048], F32) for i in range(2)]
        hb = [self.sb(st, "hb%d" % i, [128, 2048], BF16) for i in range(2)]
        hT = self.sb(st, "hT", [128, 16, 128], F32)
        lg = self.sb(st, "lg", [128, 32], F32)
        cm = self.sb(st, "rcm", [128, 32, 32], F32)
        rk = self.sb(st, "rrk", [128, 32], F32)
        sel4 = self.sb(st, "sel4", [128, 32], F32)
        selb = self.sb(st, "selb", [128, 32], BF16)
        mxv = self.sb(st, "rmx", [128, 1], F32)
        ex = self.sb(st, "rex", [128, 32], F32)
        dn = self.sb(st, "rdn", [128, 1], F32)
        carry = self.sb(st, "carry", [128, 32], F32)
        addr = self.sb(st, "addr", [128, 32], F32)
        oh = self.sb(st, "oh", [128, 32], F32)
        t32 = self.sb(st, "t32", [128, 32], F32)
        slf = self.sb(st, "slf", [128, 4], F32)
        lt = self.ln_tmp(st, "1")
        S.add("dve", lambda e: e.memset(carry[:], 0.0), writes=["carry"])
        for tt in range(16):
            b = tt % 2
            t0 = tt * 128
            self.dma("sp", mx[b][:], self.mixed[t0:t0 + 128, :], reads=["mixed"], writes=["mx%d" % b])
            self.dma("act", xt[b][:], I["x"][t0:t0 + 128, :], writes=["xres%d" % b])
            for grp in range(4):
                psv = ps[grp % 2][:].bitcast(BF16)

                def tr(e, b=b, grp=grp, psv=psv):
                    last = None
                    for i in range(4):
                        dk = grp * 4 + i
                        last = e.transpose(out=psv[:, i * 128:(i + 1) * 128], in_=mx[b][:, dk * 128:(dk + 1) * 128], identity=C["c_ident"][:])
                    return last
                S.add("pe", tr, reads=["mx%d" % b, "c_ident"], writes=["ps%d" % (grp % 2)])
                dst = mxT[b][:, grp * 4:(grp + 1) * 4, :]
                src = psv[:, 0:512].rearrange("p (a b) -> p a b", a=4)
                S.add("act", lambda e, dst=dst, src=src: e.copy(out=dst, in_=src), reads=["ps%d" % (grp % 2)], writes=["mxT%d" % b])
            for cg in range(4):
                pb = 2 + cg % 2
                pairs = [(mxT[b][:, fk, :], wo[:, fk, cg * 512:(cg + 1) * 512]) for fk in range(16)]
                self.mm_group(ps[pb][:], pairs, reads=["mxT%d" % b, "wo"], writes=["ps%d" % pb])
                S.add("dve", lambda e, cg=cg, pb=pb, b=b: e.scalar_tensor_tensor(out=pre[:, cg * 512:(cg + 1) * 512], in0=xt[b][:, cg * 512:(cg + 1) * 512],
                                                                               scalar=ALPHA, in1=ps[pb][:], op0=ALU.mult, op1=ALU.add),
                      reads=["ps%d" % pb, "xres%d" % b], writes=["pre"])
            self.layernorm(pre[:], "pre", hh[b][:], "hh%d" % b, g1, "bc_g1", b1, "bc_b1", lt)
            self.dma("sp", self.hf[t0:t0 + 128, :], hh[b][:], reads=["hh%d" % b], writes=["hf"])
            S.add("act", lambda e, b=b: e.copy(out=hb[b][:], in_=hh[b][:]), reads=["hh%d" % b], writes=["hb%d" % b])
            for grp in range(4):
                pb = 4 + grp % 2

                def trf(e, b=b, grp=grp, pb=pb):
                    last = None
                    for i in range(4):
                        dk = grp * 4 + i
                        last = e.transpose(out=ps[pb][:, i * 128:(i + 1) * 128], in_=hh[b][:, dk * 128:(dk + 1) * 128], identity=C["ident_f"][:])
                    return last
                S.add("pe", trf, reads=["hh%d" % b, "ident_f"], writes=["ps%d" % pb])
                S.add("act", lambda e, grp=grp, pb=pb: e.copy(out=hT[:, grp * 4:(grp + 1) * 4, :], in_=ps[pb][:].rearrange("p (a b) -> p a b", a=4)),
                      reads=["ps%d" % pb], writes=["hT"])
            pairs = [(hT[:, dk, :], wr[:, dk, :]) for dk in range(16)]
            self.mm_group(ps[6][:, 0:32], pairs, reads=["hT", "wr"], writes=["ps6"])
            S.add("dve", lambda e: e.tensor_tensor(out=lg[:], in0=ps[6][:, 0:32], in1=br[:], op=ALU.add), reads=["ps6", "bc_br"], writes=["lg"])
            if self.dbg == "logits":
                self.dma("sp", self.dlog[t0:t0 + 128, 0:32], lg[:], reads=["lg"], writes=["dlog"])
            S.add("dve", lambda e: e.tensor_tensor(out=cm[:], in0=lg[:].unsqueeze(1).broadcast_to([128, 32, 32]),
                                                   in1=lg[:].unsqueeze(2).broadcast_to([128, 32, 32]), op=ALU.is_gt), reads=["lg"], writes=["rcm"])
            S.add("dve", lambda e: e.tensor_reduce(out=rk[:], in_=cm[:], axis=AX.X, op=ALU.add), reads=["rcm"], writes=["rrk"])
            S.add("dve", lambda e: e.tensor_scalar(out=sel4[:], in0=rk[:], scalar1=3.5, scalar2=None, op0=ALU.is_lt), reads=["rrk"], writes=["sel4"])
            S.add("dve", lambda e: e.tensor_copy(out=selb[:], in_=sel4[:]), reads=["sel4"], writes=["selb"])
            S.add("dve", lambda e: e.tensor_reduce(out=mxv[:], in_=lg[:], axis=AX.X, op=ALU.max), reads=["lg"], writes=["rmx"])
            S.add("dve", lambda e: e.tensor_scalar(out=ex[:], in0=lg[:], scalar1=mxv[:, 0:1], scalar2=None, op0=ALU.subtract), reads=["lg", "rmx"], writes=["rex"])
            S.add("act", lambda e: e.activation(out=ex[:], in_=ex[:], func=AF.Exp), reads=["rex"], writes=["rex"])
            S.add("dve", lambda e: e.tensor_tensor(out=ex[:], in0=ex[:], in1=sel4[:], op=ALU.mult), reads=["rex", "sel4"], writes=["rex"])
            S.add("dve", lambda e: e.tensor_reduce(out=dn[:], in_=ex[:], axis=AX.X, op=ALU.add), reads=["rex"], writes=["rdn"])
            S.add("dve", lambda e: e.reciprocal(out=dn[:], in_=dn[:]), reads=["rdn"], writes=["rdn"])
            S.add("dve", lambda e: e.tensor_scalar(out=ex[:], in0=ex[:], scalar1=dn[:, 0:1], scalar2=None, op0=ALU.mult), reads=["rex", "rdn"], writes=["rex"])
            self.mm_group(ps[7][:, 0:32], [(C["c_us"][:], selb[:])], reads=["c_us", "selb"], writes=["ps7a"])
            self.mm_group(ps[7][:, 32:64], [(C["c_ones"][:], selb[:])], reads=["c_ones", "selb"], writes=["ps7b"])
            S.add("dve", lambda e: e.tensor_tensor(out=addr[:], in0=ps[7][:, 0:32], in1=carry[:], op=ALU.add), reads=["ps7a", "carry"], writes=["addr"])
            S.add("dve", lambda e: e.tensor_tensor(out=carry[:], in0=ps[7][:, 32:64], in1=carry[:], op=ALU.add), reads=["ps7b", "carry"], writes=["carry"])
            S.add("dve", lambda e: e.tensor_scalar(out=addr[:], in0=addr[:], scalar1=float(CAP - 1), scalar2=None, op0=ALU.min), reads=["addr"], writes=["addr"])
            S.add("dve", lambda e: e.tensor_tensor(out=addr[:], in0=addr[:], in1=C["c_ecap"][:], op=ALU.add), reads=["addr", "c_ecap"], writes=["addr"])
            for k in range(4):
                S.add("dve", lambda e, k=k: e.tensor_scalar(out=oh[:], in0=rk[:], scalar1=float(k), scalar2=None, op0=ALU.is_equal), reads=["rrk"], writes=["oh"])
                S.add("dve", lambda e: e.tensor_tensor(out=t32[:], in0=oh[:], in1=addr[:], op=ALU.mult), reads=["oh", "addr"], writes=["t32"])
                S.add("dve", lambda e, k=k: e.tensor_reduce(out=slf[:, k:k + 1], in_=t32[:], axis=AX.X, op=ALU.add), reads=["t32"], writes=["slf"])
                S.add("dve", lambda e: e.tensor_tensor(out=t32[:], in0=oh[:], in1=ex[:], op=ALU.mult), reads=["oh", "rex"], writes=["t32"])
                S.add("dve", lambda e, k=k, tt=tt: e.tensor_reduce(out=self.gates[:, tt, k:k + 1], in_=t32[:], axis=AX.X, op=ALU.add), reads=["t32"], writes=["gatesk"])
            S.add("dve", lambda e, tt=tt: e.tensor_copy(out=self.slots[:, tt, :], in_=slf[:]), reads=["slf"], writes=["slots"])
            if self.dbg == "logits":
                self.dma("sp", self.dlog[t0:t0 + 128, 32:36], slf[:], reads=["slf"], writes=["dlog"])
                self.dma("sp", self.dlog[t0:t0 + 128, 36:40], self.gates[:, tt, :], reads=["gatesk"], writes=["dlog"])
            for k in range(4):
                S.add("pool", lambda e, k=k, tt=tt, b=b: e.indirect_dma_start(
                    out=self.xbuf, out_offset=bass.IndirectOffsetOnAxis(ap=self.slots[:, tt, k:k + 1], axis=0),
                    in_=hb[b][:], in_offset=None),
                    reads=["hb%d" % b, "slots"], writes=["xbuf"], dma=True)
        S.add("dve", lambda e: e.tensor_copy(out=self.cnti[:], in_=carry[:]), reads=["carry"], writes=["cnti"])
        S.emit()

    def phase_experts(self, st):
        S, I, C = self.S, self.I, self.C
        ps = self.ps
        NW = 3
        NS = CAP // 128
        CW = 256
        NCH = CAP // CW
        wsl = [self.sb(st, "ew%d" % i, [128, 16, 512], BF16) for i in range(NW)]
        xin = [self.sb(st, "exin%d" % i, [128, 2048], BF16) for i in range(2)]
        XT = self.sb(st, "eXT", [128, 16, CAP], BF16)
        actT = self.sb(st, "eact", [128, 16, CAP], BF16)
        ysm = [self.sb(st, "eyo%d" % i, [128, 512], BF16) for i in range(4)]
        bgu = [self.sb(st, "ebgu%d" % i, [1, 4096], BF16) for i in range(2)]
        bdn = [self.sb(st, "ebdn%d" % i, [1, 2048], BF16) for i in range(2)]
        onesr = self.sb(st, "eones", [1, 512], BF16)
        xg = [self.sb(st, "exg%d" % i, [128, CW], F32) for i in range(2)]
        sg = [self.sb(st, "esg%d" % i, [128, CW], F32) for i in range(2)]
        tl = [self.sb(st, "etl%d" % i, [128, CW], F32) for i in range(2)]
        S.add("dve", lambda e: e.memset(onesr[:], 1.0), writes=["eones"])

        dsc = self.sb(st, "edsc", [128, 2], F32)
        asc = self.sb(st, "easc", [128, 2], F32)
        S.add("pool", lambda e: e.memset(asc[:], 0.0), writes=["easc"])

        def dskip(e):
            return e.memset(dsc[:, 0:1], 0.0)

        def askip(e):
            return e.copy(out=asc[:, 0:1], in_=asc[:, 1:2])

        def skipper(bank):
            return lambda e: e.matmul(ps[bank][:, 0:2], lhsT=onesr[0:1, 0:128], rhs=onesr[0:1, 0:2], start=True, stop=True)
        S.add("pool", lambda e: e.memset(XT[:], 0.0), writes=["eXT"])
        nw = 0
        nf = 0
        ny = 0
        ntr = 0
        nx = 0
        for ex in range(NEXP):
            b = ex % 2
            self.dma("pool", bgu[b][:], I["b_gu"][ex:ex + 1, :], writes=["ebgu%d" % b])
            self.dma("pool", bdn[b][:], I["b_dn"][ex:ex + 1, :], writes=["ebdn%d" % b])
            for c in range(NS):
                pr = (ex, c * 128)
                xb = nx % 2
                nx += 1
                self.dma("sp", xin[xb][:], self.xbuf[ex * CAP + c * 128:ex * CAP + (c + 1) * 128, :], reads=["xbuf"], writes=["exin%d" % xb])
                for grp in range(4):
                    pb = ntr % 2
                    ntr += 1
                    psv = ps[pb][:].bitcast(BF16)

                    def tr(e, xb=xb, grp=grp, psv=psv):
                        last = None
                        for i in range(4):
                            dk = grp * 4 + i
                            last = e.transpose(out=psv[:, i * 128:(i + 1) * 128], in_=xin[xb][:, dk * 128:(dk + 1) * 128], identity=C["c_ident"][:])
                        return last
                    S.add("pe", tr, reads=["exin%d" % xb, "c_ident", "eones"], writes=["ps%d" % pb], pred=(pr[0], pr[1], skipper(pb)))
                    dst = XT[:, grp * 4:(grp + 1) * 4, c * 128:(c + 1) * 128]
                    src = psv[:, 0:512].rearrange("p (a b) -> p a b", a=4)
                    S.add("act", lambda e, dst=dst, src=src: e.copy(out=dst, in_=src), reads=["ps%d" % pb, "easc"], writes=["eXT"], pred=(pr[0], pr[1], askip))
            for s in range(8):
                wb = nw % NW
                nw += 1
                self.dma("pool", wsl[wb][:], I["w_gu"][ex][:, s * 512:(s + 1) * 512].rearrange("(k p) n -> p k n", p=128), writes=["ew%d" % wb])
                for ftl in range(2):
                    ft = 2 * s + ftl
                    for ch in range(NCH):
                        pr = (ex, ch * CW)
                        fb = nf % 2
                        nf += 1
                        pg, pl = 2 + 2 * fb, 3 + 2 * fb
                        for which, pbk in ((0, pg), (1, pl)):
                            pairs = [(wsl[wb][:, dk, ftl * 256 + which:ftl * 256 + 256:2], XT[:, dk, ch * CW:(ch + 1) * CW]) for dk in range(16)]
                            pairs.append((bgu[b][0:1, ft * 256 + which:ft * 256 + 256:2], onesr[0:1, 0:CW]))
                            self.mm_group(ps[pbk][:, 0:CW], pairs, reads=["ew%d" % wb, "eXT", "ebgu%d" % b, "eones"], writes=["ps%d" % pbk], pred=(pr[0], pr[1], skipper(pbk)))
                        S.add("dve", lambda e, fb=fb, pg=pg: e.tensor_scalar(out=xg[fb][:], in0=ps[pg][:, 0:CW], scalar1=7.0, scalar2=None, op0=ALU.min),
                              reads=["ps%d" % pg], writes=["exg%d" % fb], pred=(pr[0], pr[1], dskip))
                        S.add("act", lambda e, fb=fb: e.activation(out=sg[fb][:], in_=xg[fb][:], func=AF.Sigmoid, scale=1.702),
                              reads=["exg%d" % fb, "easc"], writes=["esg%d" % fb], pred=(pr[0], pr[1], askip))
                        S.add("dve", lambda e, fb=fb, pl=pl: e.tensor_scalar(out=tl[fb][:], in0=ps[pl][:, 0:CW], scalar1=1.0, scalar2=-6.0, op0=ALU.add, op1=ALU.max),
                              reads=["ps%d" % pl], writes=["etl%d" % fb], pred=(pr[0], pr[1], dskip))
                        S.add("dve", lambda e, fb=fb: e.scalar_tensor_tensor(out=tl[fb][:], in0=tl[fb][:], scalar=8.0, in1=xg[fb][:], op0=ALU.min, op1=ALU.mult),
                              reads=["etl%d" % fb, "exg%d" % fb], writes=["etl%d" % fb], pred=(pr[0], pr[1], dskip))
                        S.add("dve", lambda e, fb=fb, ft=ft, ch=ch: e.tensor_tensor(out=actT[:, ft, ch * CW:(ch + 1) * CW], in0=tl[fb][:], in1=sg[fb][:], op=ALU.mult),
                              reads=["etl%d" % fb, "esg%d" % fb], writes=["eact"], pred=(pr[0], pr[1], dskip))
            for cg in range(4):
                wb = nw % NW
                nw += 1
                self.dma("pool", wsl[wb][:], I["w_dn"][ex][:, cg * 512:(cg + 1) * 512].rearrange("(k p) n -> p k n", p=128), writes=["ew%d" % wb])
                for c in range(NS):
                    pr = (ex, c * 128)
                    pb = 6 + ny % 2
                    yb = ny % 4
                    ny += 1
                    pairs = [(actT[:, fk, c * 128:(c + 1) * 128], wsl[wb][:, fk, :]) for fk in range(16)]
                    pairs.append((onesr[0:1, 0:128], bdn[b][0:1, cg * 512:(cg + 1) * 512]))
                    self.mm_group(ps[pb][:], pairs, reads=["ew%d" % wb, "eact", "ebdn%d" % b, "eones"], writes=["ps%d" % pb], pred=(pr[0], pr[1], skipper(pb)))
                    S.add("act", lambda e, pb=pb, yb=yb: e.copy(out=ysm[yb][:], in_=ps[pb][:]), reads=["ps%d" % pb, "easc"], writes=["eyo%d" % yb], pred=(pr[0], pr[1], askip))
                    r0 = ex * CAP + c * 128
                    self.dma("sp", self.ysb[r0:r0 + 128, cg * 512:(cg + 1) * 512], ysm[yb][:], reads=["eyo%d" % yb])
        S.emit()

    def phase_combine(self, st):
        S, I, C = self.S, self.I, self.C
        g2 = self.bcast_tile(st, "g2", I["ln2_g"], 2048)
        b2 = self.bcast_tile(st, "b2", I["ln2_b"], 2048)
        yk = [self.sb(st, "yk%d" % i, [128, 4, 2048], BF16) for i in range(2)]
        ht = [self.sb(st, "cht%d" % i, [128, 2048], F32) for i in range(2)]
        acc = self.sb(st, "cacc", [128, 2048], F32)
        ot = [self.sb(st, "cot%d" % i, [128, 2048], F32) for i in range(2)]
        lt = self.ln_tmp(st, "2")
        for tt in range(16):
            b = tt % 2
            t0 = tt * 128
            for k in range(4):
                S.add("pool", lambda e, k=k, tt=tt, b=b: e.indirect_dma_start(
                    out=yk[b][:, k, :], out_offset=None, in_=self.ysb,
                    in_offset=bass.IndirectOffsetOnAxis(ap=self.slots[:, tt, k:k + 1], axis=0)),
                    reads=["ysb", "slots"], writes=["yk%d_%d" % (b, k)], dma=True)
            self.dma("sp", ht[b][:], self.hf[t0:t0 + 128, :], reads=["hf"], writes=["cht%d" % b])
            S.add("act", lambda e, b=b: e.mul(out=acc[:], in_=ht[b][:], mul=ALPHA), reads=["cht%d" % b], writes=["cacc"])
            for k in range(4):
                S.add("dve", lambda e, k=k, tt=tt, b=b: e.scalar_tensor_tensor(out=acc[:], in0=yk[b][:, k, :], scalar=self.gates[:, tt, k:k + 1], in1=acc[:],
                                                                            op0=ALU.mult, op1=ALU.add),
                      reads=["yk%d_%d" % (b, k), "gatesk", "cacc"], writes=["cacc"])
            self.layernorm(acc[:], "cacc", ot[b][:], "cot%d" % b, g2, "bc_g2", b2, "bc_b2", lt)
            self.dma("sp", self.out[t0:t0 + 128, :], ot[b][:], reads=["cot%d" % b], writes=["out"])
        S.emit()


_CACHE = {}


def _get_nc(dbg=None):
    if dbg not in _CACHE:
        k = K(dbg)
        _CACHE[dbg] = k.build()
    return _CACHE[dbg]


def kernel(dbg=None, **inputs):
    consts = make_consts()
    nc = _get_nc(dbg)
    shared = {}
    for n, shp in IN_SHAPES.items():
        if n in ("x", "mem") or (dbg is not None and n in BIGW):
            continue
        a = np.asarray(inputs[n])
        shared[n] = np.ascontiguousarray(a.reshape(shp).astype(np.float32, copy=False))
    shared.update(consts)
    x = np.asarray(inputs["x"])
    mem = np.asarray(inputs["mem"])
    in_maps = []
    for b in range(8):
        m = dict(shared)
        m["x"] = np.ascontiguousarray(x[b])
        m["mem"] = np.ascontiguousarray(mem[b])
        in_maps.append(m)
    res = run_bass_kernel_spmd(nc, in_maps, core_ids=list(range(8)))
    if dbg is not None:
        return res.results
    return np.stack([np.asarray(r["out"]) for r in res.results], axis=0).astype(np.float32, copy=False)
```

```python
import numpy as np
from contextlib import ExitStack
import concourse.bass as bass
import concourse.mybir as mybir
from concourse.bass_utils import run_bass_kernel_spmd

F32 = mybir.dt.float32
BF16 = mybir.dt.bfloat16
I32 = mybir.dt.int32
AF = mybir.ActivationFunctionType
ALU = mybir.AluOpType
AX = mybir.AxisListType

S_LEN = 2048
D = 2048
NT = 16
CAP = 1024
NCB = CAP // 128
NEXP = 32
NEG = -30000.0
SCALE = 128 ** -0.5
ALPHA = 2.0 ** 0.25
EPS = 1e-5


class _Op:
    __slots__ = ("eng", "fn", "deps", "is_dma", "sem", "val", "needs_inc", "idx", "ninst", "pred")


class Sched:
    ENGS = ("pe", "act", "dve", "pool", "sp")

    def __init__(self, nc, lanes=None):
        self.nc = nc
        self.ops = []
        self.last_write = {}
        self.readers = {}
        self.lanes = lanes or {"sp": 8, "act": 4, "pool": 6}
        self.dma_n = {q: 0 for q in self.lanes}
        self.lane_total = {q: [0] * n for q, n in self.lanes.items()}
        self.lane_last = {q: [None] * n for q, n in self.lanes.items()}
        self.esem = {}
        self.lsem = {}
        self.cnt = {e: 0 for e in self.ENGS}
        self.nemit = 0
        self.cnt_ap = None

    def alloc_sems(self, stack):
        nc = self.nc
        for e in ("pe", "act", "dve", "pool"):
            self.esem[e] = stack.enter_context(nc.semaphore("s_" + e))
        for q, n in self.lanes.items():
            self.lsem[q] = [stack.enter_context(nc.semaphore("l_%s%d" % (q, i))) for i in range(n)]

    def add(self, eng, fn, reads=(), writes=(), dma=False, ninst=1, pred=None):
        op = _Op()
        op.pred = pred
        op.eng = eng
        op.fn = fn
        op.is_dma = dma
        op.needs_inc = False
        op.idx = len(self.ops)
        op.ninst = ninst
        op.sem = None
        op.val = 0
        deps = set()
        for r in reads:
            lw = self.last_write.get(r)
            if lw is not None:
                deps.add(lw)
        for w in writes:
            lw = self.last_write.get(w)
            if lw is not None:
                deps.add(lw)
            for rd in self.readers.get(w, ()):
                deps.add(rd)
        for r in reads:
            self.readers.setdefault(r, []).append(op.idx)
        for w in writes:
            self.last_write[w] = op.idx
            self.readers[w] = []
        if dma:
            q = eng
            n = self.dma_n[q]
            self.dma_n[q] += 1
            lane = n % self.lanes[q]
            prev = self.lane_last[q][lane]
            if prev is not None:
                deps.add(prev)
            self.lane_total[q][lane] += 16 * ninst
            op.sem = ("lane", q, lane)
            op.val = self.lane_total[q][lane]
            self.lane_last[q][lane] = op.idx
        deps.discard(op.idx)
        op.deps = deps
        for d in deps:
            self.ops[d].needs_inc = True
        self.ops.append(op)
        return op.idx

    def emit(self):
        nc = self.nc
        cnt = self.cnt
        per = {e: [] for e in self.ENGS}
        for op in self.ops:
            per[op.eng].append(op)
        for e in ("pe", "act", "dve", "pool"):
            for op in reversed(per[e]):
                if not op.is_dma:
                    op.needs_inc = True
                    break
        for op in self.ops:
            if not op.is_dma and op.needs_inc:
                cnt[op.eng] += 1
                op.val = cnt[op.eng]
                op.sem = ("eng", op.eng)

        def semh(s):
            if s[0] == "eng":
                return self.esem[s[1]]
            return self.lsem[s[1]][s[2]]

        bname = {"pe": "tensor", "act": "scalar", "dve": "vector", "pool": "gpsimd", "sp": "sync"}
        ops = self.ops
        with nc.Block() as block:
            for eng in self.ENGS:

                def body(e, eng=eng):
                    seen = {}
                    st_ = {"reg": None, "ex": None}
                    for op in per[eng]:
                        for d in sorted(op.deps):
                            dop = ops[d]
                            if dop.eng == eng and eng == "pe" and not dop.is_dma:
                                continue
                            if seen.get(dop.sem, 0) >= dop.val:
                                continue
                            e.wait_ge(semh(dop.sem), dop.val)
                            seen[dop.sem] = dop.val
                        def run(op=op):
                            ins = op.fn(e)
                            if op.is_dma:
                                if not isinstance(ins, (list, tuple)):
                                    ins = [ins]
                                assert len(ins) == op.ninst, (len(ins), op.ninst)
                                for i_ in ins:
                                    i_.then_inc(semh(op.sem), 16)
                            elif op.needs_inc:
                                if isinstance(ins, (list, tuple)):
                                    ins = ins[-1]
                                ins.then_inc(semh(op.sem), 1)
                        if op.pred is None:
                            run()
                        else:
                            ex_, thr_, skip_fn = op.pred
                            if st_["reg"] is None:
                                st_["reg"] = e.alloc_register("pr_%s_%d" % (eng, self.nemit))
                            if st_["ex"] != ex_:
                                e.reg_load(st_["reg"], self.cnt_ap[0:1, ex_:ex_ + 1])
                                st_["ex"] = ex_
                            with e.If_lt(st_["reg"], thr_ + 1):
                                ins_ = skip_fn(e)
                                assert not op.is_dma
                                if op.needs_inc:
                                    ins_.then_inc(semh(op.sem), 1)
                            with e.Else():
                                run()
                    for q in self.lanes:
                        for lane in range(self.lanes[q]):
                            v = self.lane_total[q][lane]
                            if v > 0 and seen.get(("lane", q, lane), 0) < v:
                                e.wait_ge(self.lsem[q][lane], v)
                    for e2 in ("pe", "act", "dve", "pool"):
                        if e2 != eng and cnt[e2] > 0 and seen.get(("eng", e2), 0) < cnt[e2]:
                            e.wait_ge(self.esem[e2], cnt[e2])

                getattr(block, bname[eng])(body)
        self.ops = []
        self.last_write = {}
        self.readers = {}
        self.lane_last = {q: [None] * n for q, n in self.lanes.items()}
        self.nemit += 1


def make_consts():
    c = {}
    p = np.arange(128)
    c["c_ident"] = np.eye(128, dtype=np.float32)
    cb = np.where(p[:, None] <= p[None, :], 0.0, NEG).astype(np.float32)
    fb = np.where(p[:, None] > p[None, :], 0.0, NEG).astype(np.float32)
    c["c_cb"] = np.tile(cb, (1, 4))
    c["c_fb"] = np.tile(fb, (1, 4))
    cc = np.arange(128)[:, None, None]
    tt = np.arange(16)[None, :, None]
    tl = np.arange(128)[None, None, :]
    c["c_cm"] = ((16 * cc + 31 <= 128 * tt + tl) & (cc < 127)).astype(np.float32)
    cs = np.arange(128) * 16
    ss = np.arange(32) * 64
    ov = np.clip(np.minimum(cs[:, None] + 32, ss[None, :] + 64) - np.maximum(cs[:, None], ss[None, :]), 0, None) // 16
    ov[127, :] = 0
    aux = np.zeros((128, 33), np.float32)
    aux[:127, 0] = 1.0
    aux[:, 1:] = ov
    c["c_aux"] = aux
    j = np.arange(32)[:, None, None]
    kt = np.arange(16)[None, :, None]
    k = np.arange(128)[None, None, :]
    c["c_E"] = (j == 2 * kt + k // 64).astype(np.float32)
    t = (np.arange(16)[None, :, None] * 128 + np.arange(128)[:, None, None])
    jj = np.arange(32)[None, None, :]
    cur = t // 64
    valid = jj <= cur
    forced = (jj == 0) | (jj == cur) | (jj == cur - 1)
    c["c_m1"] = (valid & ~forced).astype(np.float32)
    c["c_m2"] = np.where(forced, 1e9, np.where(valid, 0.0, -1e9)).astype(np.float32)
    c["c_us"] = (p[:, None] < p[None, :]).astype(np.float32)
    c["c_ones"] = np.ones((128, 128), np.float32)
    c["c_ecap"] = np.tile((np.arange(32) * CAP).astype(np.float32)[None, :], (128, 1))
    corr = np.ones((128, 4, 16), np.float32)
    for g, w in enumerate((2, 4, 8, 16)):
        for t_ in range(16):
            corr[:, g, t_] = w / min(t_ + 1, w)
    c["c_corr"] = corr
    return c


CONST_SHAPES = {
    "c_ident": [128, 128], "c_cb": [128, 512], "c_fb": [128, 512], "c_cm": [128, 16, 128], "c_aux": [128, 33],
    "c_E": [32, 16, 128], "c_m1": [128, 16, 32], "c_m2": [128, 16, 32], "c_us": [128, 128], "c_ones": [128, 128],
    "c_ecap": [128, 32], "c_corr": [128, 4, 16],
}

IN_SHAPES = {
    "x": [2048, 2048], "mem": [256, 2048], "w_in": [2048, 3608],
    "cmp_pos_k": [32, 128], "cmp_w1_k": [4096, 256], "cmp_w2_k": [256, 128],
    "cmp_pos_v": [32, 128], "cmp_w1_v": [4096, 256], "cmp_w2_v": [256, 128],
    "pool_w": [4, 128, 128], "pool_scale": [1, 512], "w_mem_kv": [2048, 1024],
    "gn_nsa": [1, 1024], "gn_pool": [1, 512], "gn_mem": [1, 512], "w_out": [2048, 2048],
    "ln1_g": [1, 2048], "ln1_b": [1, 2048], "w_router": [2048, 32], "b_router": [1, 32],
    "w_gu": [32, 2048, 4096], "b_gu": [32, 4096], "w_dn": [32, 2048, 2048], "b_dn": [32, 2048],
    "ln2_g": [1, 2048], "ln2_b": [1, 2048],
}

BIGW = ("w_gu", "b_gu", "w_dn", "b_dn")


class K:
    def __init__(self, dbg=None):
        self.nsa_br = (0, 1, 2)
        if dbg in ("mixed_c", "mixed_s", "mixed_w"):
            self.nsa_br = ({"c": 0, "s": 1, "w": 2}[dbg[-1]],)
            dbg = "mixed"
        self.dbg = dbg
        nc = self.nc = bass.Bass("TRN2", target_bir_lowering=False)
        self.I = {}
        for n, shp in IN_SHAPES.items():
            if dbg is not None and n in BIGW:
                continue
            self.I[n] = nc.dram_tensor(n, shp, F32, kind="ExternalInput").ap()
        for n, shp in CONST_SHAPES.items():
            self.I[n] = nc.dram_tensor(n, shp, F32, kind="ExternalInput").ap()
        self.out = nc.dram_tensor("out", [2048, 2048], F32, kind="ExternalOutput").ap()
        mk = "ExternalOutput" if dbg in ("pm", "mixed") else "Internal"
        self.mixed = nc.dram_tensor("mixed", [2048, 2048], BF16, kind=mk).ap()
        self.hf = nc.dram_tensor("hf", [2048, 2048], F32, kind=("ExternalOutput" if dbg == "h" else "Internal")).ap()
        self.xbuf = nc.dram_tensor("xbuf", [NEXP * CAP, 2048], BF16, kind="Internal").ap()
        self.ysb = nc.dram_tensor("ysb", [NEXP * CAP, 2048], BF16, kind="Internal").ap()
        if dbg == "cmp":
            self.dkc = nc.dram_tensor("dkc", [128, 256], F32, kind="ExternalOutput").ap()
            self.dvc = nc.dram_tensor("dvc", [128, 322], F32, kind="ExternalOutput").ap()
        if dbg == "logits":
            self.dlog = nc.dram_tensor("dlog", [2048, 40], F32, kind="ExternalOutput").ap()

    def sb(self, st, name, shape, dt):
        return st.enter_context(self.nc.sbuf_tensor(name, shape, dt))

    def dma(self, q, out, in_, reads=(), writes=(), pred=None):
        self.S.add(q, lambda e: e.dma_start(out=out, in_=in_), reads=reads, writes=writes, dma=True, pred=pred)

    def mm_group(self, out, pairs, reads, writes, pred=None):
        def fn(e):
            last = None
            n = len(pairs)
            for i, (l, r) in enumerate(pairs):
                last = e.matmul(out, lhsT=l, rhs=r, start=(i == 0), stop=(i == n - 1))
            return last
        self.S.add("pe", fn, reads=reads, writes=writes, pred=pred)

    def build(self):
        nc = self.nc
        with ExitStack() as st:
            S = self.S = Sched(nc)
            S.alloc_sems(st)
            self._persist = st
            self.ps = [st.enter_context(nc.psum_tensor("ps%d" % i, [128, 512], F32)) for i in range(8)]
            self.load_consts(st)
            with ExitStack() as st1:
                self.phase_xT(st1)
                with ExitStack() as st2:
                    self.phase_pool(st2)
                with ExitStack() as st2:
                    self.phase_mem(st2)
                if self.dbg == "pm":
                    return nc
                with ExitStack() as stn:
                    self._nsa = stn
                    self.qT = self.sb(stn, "qT", [128, 8, 2048], BF16)
                    self.kcT = self.sb(stn, "kcT", [128, 2, 2048], BF16)
                    self.vcT = self.sb(stn, "vcT", [128, 2, 2048], BF16)
                    self.ksT = self.sb(stn, "ksT", [128, 2, 2048], BF16)
                    self.kwT = self.sb(stn, "kwT", [128, 2, 2048], BF16)
                    self.vs = self.sb(stn, "vs", [128, 16, 2, 129], BF16)
                    self.vw = self.sb(stn, "vw", [128, 16, 2, 129], BF16)
                    self.gsig = self.sb(stn, "gsig", [128, 16, 24], F32)
                    self.kccT = self.sb(stn, "kccT", [128, 2, 128], BF16)
                    self.vcc = self.sb(stn, "vcc", [128, 2, 161], BF16)
                    self.phase_qkv(stn)
                    with ExitStack() as st2:
                        self.phase_compress(st2)
                    if self.dbg == "cmp":
                        self.dma("pool", self.dkc, self.kccT[:].rearrange("p a b -> p (a b)"))
                        self.dma("pool", self.dvc, self.vcc[:].rearrange("p a b -> p (a b)"))
                        self.S.emit()
                        return nc
                    with ExitStack() as st2:
                        self.phase_nsa(st2)
            if self.dbg == "mixed":
                return nc
            with ExitStack() as st1:
                self.phase_wout(st1)
            if self.dbg in ("h", "logits"):
                return nc
            with ExitStack() as st1:
                self.phase_experts(st1)
            with ExitStack() as st1:
                self.phase_combine(st1)
        return nc

    def load_consts(self, st):
        S, I = self.S, self.I
        C = self.C = {}
        bf_list = ["c_ident", "c_cb", "c_fb", "c_cm", "c_aux", "c_E", "c_us", "c_ones"]
        for n in bf_list:
            C[n] = self.sb(st, "k" + n, CONST_SHAPES[n], BF16)
            self.dma("pool", C[n][:], I[n], writes=[n])
        for n in ["c_m1", "c_m2", "c_ecap", "c_corr"]:
            C[n] = self.sb(st, "k" + n, CONST_SHAPES[n], F32)
            self.dma("sp", C[n][:], I[n], writes=[n])
        C["ident_f"] = self.sb(st, "kidf", [128, 128], F32)
        self.dma("sp", C["ident_f"][:], I["c_ident"], writes=["ident_f"])
        S.emit()

    def transpose_rows(self, src_dram, ntile, dstT, st, tag):
        S, C = self.S, self.C
        xb = [self.sb(st, "xb%s%d" % (tag, i), [128, 2048], BF16) for i in range(2)]
        n = 0
        for tt in range(ntile):
            b = tt % 2
            self.dma("pool", xb[b][:], src_dram[tt * 128:(tt + 1) * 128, :], writes=["xb%d" % b])
            for grp in range(4):
                pb = n % 2
                n += 1
                psv = self.ps[pb][:].bitcast(BF16)

                def tr(e, b=b, grp=grp, psv=psv):
                    last = None
                    for i in range(4):
                        dk = grp * 4 + i
                        last = e.transpose(out=psv[:, i * 128:(i + 1) * 128], in_=xb[b][:, dk * 128:(dk + 1) * 128],
                                           identity=C["c_ident"][:])
                    return last
                S.add("pe", tr, reads=["xb%d" % b, "c_ident"], writes=["ps%d" % pb])
                dst = dstT[:, grp * 4:(grp + 1) * 4, tt * 128:(tt + 1) * 128]
                src = psv[:, 0:512].rearrange("p (a b) -> p a b", a=4)
                if n % 2 == 0:
                    S.add("act", lambda e, dst=dst, src=src: e.copy(out=dst, in_=src), reads=["ps%d" % pb], writes=[tag + "T"])
                else:
                    S.add("dve", lambda e, dst=dst, src=src: e.tensor_copy(out=dst, in_=src), reads=["ps%d" % pb], writes=[tag + "T"])

    def phase_xT(self, st):
        self.xT = self.sb(st, "xT", [128, 16, 2048], BF16)
        self.memT = self.sb(st, "memT", [128, 16, 256], BF16)
        with ExitStack() as st2:
            self.transpose_rows(self.I["x"], 16, self.xT, st2, "x")
            self.transpose_rows(self.I["mem"], 2, self.memT, st2, "mem")
            self.S.emit()

    def load_slab(self, dst, src_cols, name, q="pool"):
        self.dma(q, dst, src_cols.rearrange("(k p) n -> p k n", p=128), writes=[name])

    def proj_fm(self, wsl, wname, c0, dstT_ap_fn, dname, psbase=0, nps=4):
        S = self.S
        for tq in range(4):
            pb = psbase + (self._pn % nps)
            self._pn += 1
            pairs = [(wsl[:, dk, c0:c0 + 128], self.xT[:, dk, tq * 512:(tq + 1) * 512]) for dk in range(16)]
            self.mm_group(self.ps[pb][:], pairs, reads=[wname, "xT"], writes=["ps%d" % pb])
            dst = dstT_ap_fn(tq)
            if self._pn % 2 == 0:
                S.add("act", lambda e, dst=dst, pb=pb: e.copy(out=dst, in_=self.ps[pb][:]), reads=["ps%d" % pb], writes=[dname])
            else:
                S.add("dve", lambda e, dst=dst, pb=pb: e.tensor_copy(out=dst, in_=self.ps[pb][:]), reads=["ps%d" % pb], writes=[dname])

    def rmsnorm_store(self, src, width, gn_tile, gname, outbf, oname, dram_dst, st_tmp, key):
        S = self.S
        junk, ss, sq, rstd = st_tmp
        S.add("act", lambda e: e.activation(out=junk[:, 0:width], in_=src, func=AF.Square, accum_out=ss[:, 0:1]),
              reads=[key], writes=["rn_junk", "rn_ss"])
        S.add("act", lambda e: e.activation(out=sq[:, 0:1], in_=ss[:, 0:1], func=AF.Sqrt, scale=1.0 / width, bias=EPS),
              reads=["rn_ss"], writes=["rn_sq"])
        S.add("dve", lambda e: e.reciprocal(out=rstd[:, 0:1], in_=sq[:, 0:1]), reads=["rn_sq"], writes=["rn_rstd"])
        S.add("dve", lambda e: e.scalar_tensor_tensor(out=outbf, in0=src, scalar=rstd[:, 0:1], in1=gn_tile,
                                                      op0=ALU.mult, op1=ALU.mult),
              reads=[key, "rn_rstd", gname], writes=[oname])
        self.dma("sp", dram_dst, outbf, reads=[oname], writes=["mixed"])

    def rn_tmp(self, st, tag):
        junk = self.sb(st, "rnj" + tag, [128, 1024], F32)
        ss = self.sb(st, "rns" + tag, [128, 1], F32)
        sq = self.sb(st, "rnq" + tag, [128, 1], F32)
        rstd = self.sb(st, "rnr" + tag, [128, 1], F32)
        return (junk, ss, sq, rstd)

    def bcast_tile(self, st, name, src, width, q="sp"):
        t = self.sb(st, "bc_" + name, [128, width], F32)
        self.dma(q, t[:], src.partition_broadcast(128)[:, 0, :], writes=["bc_" + name])
        return t

    def phase_pool(self, st):
        S, I, C = self.S, self.I, self.C
        self._pn = 0
        wsl = self.sb(st, "wsl_p", [128, 16, 512], BF16)
        self.load_slab(wsl[:], I["w_in"][:, 2584:3096], "wsl_p")
        pw = self.sb(st, "pw", [128, 4, 128], BF16)
        self.dma("pool", pw[:], I["pool_w"].rearrange("g c d -> c g d"), writes=["pw"])
        psc = self.bcast_tile(st, "psc", I["pool_scale"], 512)
        gnp = self.bcast_tile(st, "gnp", I["gn_pool"], 512)
        PADW = 16
        up = self.sb(st, "up", [128, 4, PADW + 2048], F32)
        ta = self.sb(st, "pl_a", [128, PADW + 2048], F32)
        tb = self.sb(st, "pl_b", [128, PADW + 2048], F32)
        pooled = self.sb(st, "pooled", [128, 4, 2048], BF16)
        S.add("pool", lambda e: e.memset(up[:], 0.0), writes=["up"])
        S.add("pool", lambda e: e.memset(ta[:], 0.0), writes=["pl_a"])
        S.add("pool", lambda e: e.memset(tb[:], 0.0), writes=["pl_b"])
        for g in range(4):
            self.proj_fm(wsl, "wsl_p", g * 128, lambda tq, g=g: up[:, g, PADW + tq * 512:PADW + (tq + 1) * 512], "up")
        W = PADW + 2048
        for g, w in enumerate((2, 4, 8, 16)):
            src = up[:, g, :]
            sh = 1
            bufs = [ta, tb]
            bi = 0
            cur = src
            curname = "up"
            while sh < w:
                dst = bufs[bi]
                dname = "pl_a" if bi == 0 else "pl_b"
                S.add("dve", lambda e, dst=dst, cur=cur, sh=sh: e.tensor_tensor(out=dst[:, sh:W], in0=cur[:, sh:W], in1=cur[:, 0:W - sh], op=ALU.add),
                      reads=[curname], writes=[dname])
                cur = dst
                curname = dname
                bi ^= 1
                sh *= 2
            other = bufs[bi]
            oname = "pl_a" if bi == 0 else "pl_b"
            S.add("dve", lambda e, other=other, cur=cur, w=w: e.tensor_scalar(out=other[:, PADW:W], in0=cur[:, PADW:W], scalar1=1.0 / w, scalar2=None, op0=ALU.mult),
                  reads=[curname], writes=[oname])
            S.add("dve", lambda e, other=other, g=g: e.tensor_tensor(out=other[:, PADW:PADW + 16], in0=other[:, PADW:PADW + 16], in1=C["c_corr"][:, g, :], op=ALU.mult),
                  reads=[oname, "c_corr"], writes=[oname])
            S.add("dve", lambda e, other=other, g=g: e.tensor_tensor(out=pooled[:, g, :], in0=other[:, PADW:W], in1=up[:, g, PADW:W], op=ALU.subtract),
                  reads=[oname, "up"], writes=["pooled"])
        tmp = self.rn_tmp(st, "p")
        ot = [self.sb(st, "po%d" % i, [128, 512], F32) for i in range(2)]
        ob = [self.sb(st, "pob%d" % i, [128, 512], BF16) for i in range(2)]
        for tt in range(16):
            pb = 4 + tt % 2
            b = tt % 2

            def fn(e, tt=tt, pb=pb):
                last = None
                for g in range(4):
                    last = e.matmul(self.ps[pb][:, g * 128:(g + 1) * 128], lhsT=pooled[:, g, tt * 128:(tt + 1) * 128], rhs=pw[:, g, :], start=True, stop=True)
                return last
            S.add("pe", fn, reads=["pooled", "pw"], writes=["ps%d" % pb])
            S.add("dve", lambda e, b=b, pb=pb: e.tensor_tensor(out=ot[b][:], in0=self.ps[pb][:], in1=psc[:], op=ALU.mult),
                  reads=["ps%d" % pb, "bc_psc"], writes=["po%d" % b])
            self.rmsnorm_store(ot[b][:], 512, gnp[:], "bc_gnp", ob[b][:], "pob%d" % b,
                               self.mixed[tt * 128:(tt + 1) * 128, 1024:1536], tmp, "po%d" % b)
        S.emit()

    def phase_mem(self, st):
        S, I, C = self.S, self.I, self.C
        self._pn = 0
        wsl = self.sb(st, "wsl_m", [128, 16, 512], BF16)
        self.load_slab(wsl[:], I["w_in"][:, 3096:3608], "wsl_m")
        wk = self.sb(st, "wmk", [128, 16, 512], BF16)
        wv = self.sb(st, "wmv", [128, 16, 512], BF16)
        self.load_slab(wk[:], I["w_mem_kv"][:, 0:512], "wmk")
        self.load_slab(wv[:], I["w_mem_kv"][:, 512:1024], "wmv")
        gnm = self.bcast_tile(st, "gnm", I["gn_mem"], 512)
        qmT = self.sb(st, "qmT", [128, 4, 2048], BF16)
        kmT = self.sb(st, "kmT", [128, 4, 256], BF16)
        vm = self.sb(st, "vm", [128, 2, 512], BF16)
        for h in range(4):
            self.proj_fm(wsl, "wsl_m", h * 128, lambda tq, h=h: qmT[:, h, tq * 512:(tq + 1) * 512], "qmT")
        for h in range(4):
            pb = h % 2
            pairs = [(wk[:, dk, h * 128:(h + 1) * 128], self.memT[:, dk, :]) for dk in range(16)]
            self.mm_group(self.ps[pb][:, 0:256], pairs, reads=["wmk", "memT"], writes=["ps%d" % pb])
            S.add("act", lambda e, h=h, pb=pb: e.copy(out=kmT[:, h, :], in_=self.ps[pb][:, 0:256]), reads=["ps%d" % pb], writes=["kmT"])
        for mt in range(2):
            pb = 2 + mt
            pairs = [(self.memT[:, dk, mt * 128:(mt + 1) * 128], wv[:, dk, :]) for dk in range(16)]
            self.mm_group(self.ps[pb][:], pairs, reads=["wmv", "memT"], writes=["ps%d" % pb])
            S.add("dve", lambda e, mt=mt, pb=pb: e.tensor_copy(out=vm[:, mt, :], in_=self.ps[pb][:]), reads=["ps%d" % pb], writes=["vm"])
        pT = [self.sb(st, "mpT%d" % i, [128, 512], BF16) for i in range(4)]
        oacc = [self.sb(st, "moa%d" % i, [128, 4, 512], F32) for i in range(2)]
        den = self.sb(st, "mden", [128, 4], F32)
        rden = self.sb(st, "mrden", [128, 4], F32)
        tmp = self.rn_tmp(st, "m")
        ob = [self.sb(st, "mob%d" % i, [128, 512], BF16) for i in range(2)]
        n = 0
        for tq in range(4):
            oa = oacc[tq % 2]
            oan = "moa%d" % (tq % 2)
            for h in range(4):
                pts = []
                for mt in range(2):
                    pb = n % 2
                    pi = n % 4
                    n += 1
                    self.mm_group(self.ps[pb][:], [(kmT[:, h, mt * 128:(mt + 1) * 128], qmT[:, h, tq * 512:(tq + 1) * 512])],
                                  reads=["kmT", "qmT"], writes=["ps%d" % pb])
                    S.add("act", lambda e, pi=pi, pb=pb: e.activation(out=pT[pi][:], in_=self.ps[pb][:], func=AF.Exp, scale=SCALE),
                          reads=["ps%d" % pb], writes=["mpT%d" % pi])
                    pts.append(pi)
                ob_ = 4 + (tq * 4 + h) % 2
                db_ = 6

                def pv(e, pts=pts, h=h, ob_=ob_):
                    last = None
                    for tl in range(4):
                        for mt in range(2):
                            last = e.matmul(self.ps[ob_][:, tl * 128:(tl + 1) * 128], lhsT=pT[pts[mt]][:, tl * 128:(tl + 1) * 128],
                                            rhs=vm[:, mt, h * 128:(h + 1) * 128], start=(mt == 0), stop=(mt == 1))
                    for tl in range(4):
                        for mt in range(2):
                            last = e.matmul(self.ps[6][:, tl:tl + 1], lhsT=pT[pts[mt]][:, tl * 128:(tl + 1) * 128],
                                            rhs=C["c_ones"][:, 0:1], start=(mt == 0), stop=(mt == 1))
                    return last
                S.add("pe", pv, reads=["mpT%d" % pts[0], "mpT%d" % pts[1], "vm", "c_ones"], writes=["ps%d" % ob_, "ps6"])
                S.add("dve", lambda e: e.reciprocal(out=rden[:, 0:4], in_=self.ps[6][:, 0:4]), reads=["ps6"], writes=["mrden"])
                for tl in range(4):
                    S.add("dve", lambda e, tl=tl, h=h, oa=oa, ob_=ob_: e.tensor_scalar(
                        out=oa[:, tl, h * 128:(h + 1) * 128], in0=self.ps[ob_][:, tl * 128:(tl + 1) * 128],
                        scalar1=rden[:, tl:tl + 1], scalar2=None, op0=ALU.mult),
                        reads=["ps%d" % ob_, "mrden"], writes=[oan])
            for tl in range(4):
                tt = tq * 4 + tl
                b = tt % 2
                self.rmsnorm_store(oa[:, tl, :], 512, gnm[:], "bc_gnm", ob[b][:], "mob%d" % b,
                                   self.mixed[tt * 128:(tt + 1) * 128, 1536:2048], tmp, oan)
        S.emit()

    def phase_qkv(self, st):
        S, I = self.S, self.I
        self._pn = 0
        S.add("pool", lambda e: e.memset(self.vs[:, :, :, 128:129], 1.0), writes=["vs"])
        S.add("pool", lambda e: e.memset(self.vw[:, :, :, 128:129], 1.0), writes=["vw"])
        for h_ in range(2):
            S.add("dve", lambda e, h_=h_: e.tensor_copy(out=self.vcc[:, h_, 128:161], in_=self.C["c_aux"][:]), reads=["c_aux"], writes=["vcc"])
        with ExitStack() as st2:
            wsl = [self.sb(st2, "wsl_q%d" % i, [128, 16, 512], BF16) for i in range(2)]
            wg = self.sb(st2, "wsl_g", [128, 16, 24], BF16)
            self.load_slab(wg[:], I["w_in"][:, 2560:2584], "wsl_g")
            for si in range(5):
                b = si % 2
                wn = "wsl_q%d" % b
                self.load_slab(wsl[b][:], I["w_in"][:, si * 512:(si + 1) * 512], wn)
                if si < 2:
                    for hh in range(4):
                        self.proj_fm(wsl[b], wn, hh * 128, lambda tq, hd=si * 4 + hh: self.qT[:, hd, tq * 512:(tq + 1) * 512], "qT")
                elif si == 2:
                    for h in range(2):
                        self.proj_fm(wsl[b], wn, h * 128, lambda tq, h=h: self.kcT[:, h, tq * 512:(tq + 1) * 512], "kcT")
                        self.proj_fm(wsl[b], wn, 256 + h * 128, lambda tq, h=h: self.vcT[:, h, tq * 512:(tq + 1) * 512], "vcT")
                else:
                    kT = self.ksT if si == 3 else self.kwT
                    kn = "ksT" if si == 3 else "kwT"
                    vv = self.vs if si == 3 else self.vw
                    vn = "vs" if si == 3 else "vw"
                    for h in range(2):
                        self.proj_fm(wsl[b], wn, h * 128, lambda tq, h=h, kT=kT: kT[:, h, tq * 512:(tq + 1) * 512], kn)
                    for tt in range(16):
                        pb = 4 + tt % 2
                        pairs = [(self.xT[:, dk, tt * 128:(tt + 1) * 128], wsl[b][:, dk, 256:512]) for dk in range(16)]
                        self.mm_group(self.ps[pb][:, 0:256], pairs, reads=[wn, "xT"], writes=["ps%d" % pb])
                        S.add("act" if tt % 2 else "dve",
                              (lambda e, tt=tt, pb=pb, vv=vv: e.copy(out=vv[:, tt, :, 0:128], in_=self.ps[pb][:, 0:256].rearrange("p (a b) -> p a b", a=2))) if tt % 2 else
                              (lambda e, tt=tt, pb=pb, vv=vv: e.tensor_copy(out=vv[:, tt, :, 0:128], in_=self.ps[pb][:, 0:256].rearrange("p (a b) -> p a b", a=2))),
                              reads=["ps%d" % pb], writes=[vn])
            for tt in range(16):
                pb = 6 + tt % 2
                pairs = [(self.xT[:, dk, tt * 128:(tt + 1) * 128], wg[:, dk, :]) for dk in range(16)]
                self.mm_group(self.ps[pb][:, 0:24], pairs, reads=["wsl_g", "xT"], writes=["ps%d" % pb])
                S.add("act", lambda e, tt=tt, pb=pb: e.activation(out=self.gsig[:, tt, :], in_=self.ps[pb][:, 0:24], func=AF.Sigmoid),
                      reads=["ps%d" % pb], writes=["gsig"])
            S.emit()

    def phase_compress(self, st_unused):
        S, I, C = self.S, self.I, self.C
        for kind in range(2):
            with ExitStack() as st2:
                sfx = "k" if kind == 0 else "v"
                src = self.kcT if kind == 0 else self.vcT
                srcn = "kcT" if kind == 0 else "vcT"
                w1 = self.sb(st2, "cw1" + sfx, [128, 32, 256], BF16)
                self.dma("pool", w1[:], I["cmp_w1_" + sfx].rearrange("(l d) n -> d l n", d=128), writes=["cw1" + sfx])
                w2 = self.sb(st2, "cw2" + sfx, [128, 2, 128], BF16)
                self.dma("pool", w2[:], I["cmp_w2_" + sfx].rearrange("(a p) d -> p a d", p=128), writes=["cw2" + sfx])
                pos = self.sb(st2, "cpos" + sfx, [32, 128], F32)
                self.dma("sp", pos[:], I["cmp_pos_" + sfx], writes=["cpos" + sfx])
                posT = self.sb(st2, "cposT" + sfx, [128, 32], BF16)
                S.add("pe", lambda e, pos=pos: e.transpose(out=self.ps[7][:, 0:32], in_=pos[:], identity=C["ident_f"][0:32, 0:32]),
                      reads=["cpos" + sfx, "ident_f"], writes=["ps7"])
                S.add("dve", lambda e, posT=posT: e.tensor_copy(out=posT[:], in_=self.ps[7][:, 0:32]), reads=["ps7"], writes=["cposT" + sfx])
                bias = self.sb(st2, "cbias" + sfx, [128, 2], F32)
                for half in range(2):
                    pairs = [(w1[:, l, half * 128:(half + 1) * 128], posT[:, l:l + 1]) for l in range(32)]
                    self.mm_group(self.ps[6][:, half:half + 1], pairs, reads=["cw1" + sfx, "cposT" + sfx], writes=["ps6"])
                S.add("dve", lambda e, bias=bias: e.tensor_copy(out=bias[:], in_=self.ps[6][:, 0:2]), reads=["ps6"], writes=["cbias" + sfx])
                xs = self.sb(st2, "cxs" + sfx, [128, 128], F32)
                x2 = self.sb(st2, "cx2" + sfx, [128, 128], F32)
                sg = self.sb(st2, "csg" + sfx, [128, 128], F32)
                gT = [self.sb(st2, "cg%s%d" % (sfx, i), [128, 128], BF16) for i in range(2)]
                for h in range(2):
                    for half in range(2):
                        pb = half
                        pairs = [(w1[:, l, half * 128:(half + 1) * 128], src[:, h, l:l + 16 * 126 + 1:16]) for l in range(32)]
                        self.mm_group(self.ps[pb][:, 0:127], pairs, reads=["cw1" + sfx, srcn], writes=["ps%d" % pb])
                        S.add("act", lambda e, pb=pb, half=half, xs=xs, bias=bias: e.activation(out=xs[:, 0:127], in_=self.ps[pb][:, 0:127], func=AF.Identity,
                                                                             bias=bias[:, half:half + 1]),
                              reads=["ps%d" % pb, "cbias" + sfx], writes=["cxs"])
                        S.add("dve", lambda e, xs=xs, x2=x2: e.tensor_tensor(out=x2[:, 0:127], in0=xs[:, 0:127], in1=xs[:, 0:127], op=ALU.mult),
                              reads=["cxs"], writes=["cx2"])
                        S.add("dve", lambda e, x2=x2: e.tensor_scalar(out=x2[:, 0:127], in0=x2[:, 0:127], scalar1=0.044715, scalar2=1.0, op0=ALU.mult, op1=ALU.add),
                              reads=["cx2"], writes=["cx2"])
                        S.add("dve", lambda e, xs=xs, x2=x2: e.tensor_tensor(out=x2[:, 0:127], in0=x2[:, 0:127], in1=xs[:, 0:127], op=ALU.mult),
                              reads=["cx2", "cxs"], writes=["cx2"])
                        S.add("act", lambda e, x2=x2, sg=sg: e.activation(out=sg[:, 0:127], in_=x2[:, 0:127], func=AF.Sigmoid, scale=1.5957691216057308),
                              reads=["cx2"], writes=["csg"])
                        S.add("dve", lambda e, xs=xs, sg=sg, half=half, gT=gT: e.tensor_tensor(out=gT[half][:, 0:127], in0=xs[:, 0:127], in1=sg[:, 0:127], op=ALU.mult),
                              reads=["cxs", "csg"], writes=["cg%d" % half])
                    if kind == 0:
                        pairs = [(w2[:, half, :], gT[half][:, 0:127]) for half in range(2)]
                        self.mm_group(self.ps[2][:, 0:127], pairs, reads=["cw2" + sfx, "cg0", "cg1"], writes=["ps2"])
                        S.add("act", lambda e, h=h: e.copy(out=self.kccT[:, h, 0:127], in_=self.ps[2][:, 0:127]), reads=["ps2"], writes=["kccT"])
                    else:
                        pairs = [(gT[half][:, 0:127], w2[:, half, :]) for half in range(2)]
                        self.mm_group(self.ps[2][0:127, 0:128], pairs, reads=["cw2" + sfx, "cg0", "cg1"], writes=["ps2"])
                        S.add("act", lambda e, h=h: e.copy(out=self.vcc[0:127, h, 0:128], in_=self.ps[2][0:127, 0:128]), reads=["ps2"], writes=["vcc"])
                S.emit()

    def phase_nsa(self, st):
        S, I, C = self.S, self.I, self.C
        ps = self.ps
        gnn = self.bcast_tile(st, "gnn", I["gn_nsa"], 1024)
        ec = [self.sb(st, "ec%d" % i, [128, 512], BF16) for i in range(2)]
        pT = [self.sb(st, "npT%d" % i, [128, 512], BF16) for i in range(3)]
        oacc = [self.sb(st, "noa%d" % i, [128, 8, 128], F32) for i in range(2)]
        ob = [self.sb(st, "nob%d" % i, [128, 1024], BF16) for i in range(2)]
        rc = self.sb(st, "nrc", [128, 4], F32)
        coef = self.sb(st, "ncoef", [128, 4], F32)
        impg = self.sb(st, "nimpg", [128, 4, 32], F32)
        imp = self.sb(st, "nimp", [128, 32], F32)
        cmpm = self.sb(st, "ncmpm", [128, 32, 32], F32)
        rk = self.sb(st, "nrk", [128, 32], F32)
        nsel = self.sb(st, "nsel", [128, 32], BF16)
        nselT = [self.sb(st, "nselT%d" % i, [32, 128], BF16) for i in range(2)]
        tmp = self.rn_tmp(st, "n")
        nst = 0
        npt = 0
        nsl = 0
        for tt in range(16):
            t0 = tt * 128
            oa = oacc[tt % 2]
            oan = "noa%d" % (tt % 2)
            for h in range(2):
                Q = self.qT[:, 4 * h:4 * h + 4, t0:t0 + 128]
                ncv = min(127, 8 * tt + 7)
                pb = nst % 3
                nst += 1
                self.mm_group(ps[pb][0:ncv, :], [(self.kccT[:, h, 0:ncv], Q)], reads=["kccT", "qT"], writes=["ps%d" % pb])
                eb = (tt * 2 + h) % 2
                S.add("act", lambda e, eb=eb, pb=pb, ncv=ncv: e.activation(out=ec[eb][0:ncv, :], in_=ps[pb][0:ncv, :], func=AF.Exp, scale=SCALE),
                      reads=["ps%d" % pb], writes=["ec%d" % eb])
                S.add("pool", lambda e, eb=eb, ncv=ncv, tt=tt: e.tensor_tensor(
                    out=ec[eb][0:ncv, :].rearrange("p (a b) -> p a b", a=4), in0=ec[eb][0:ncv, :].rearrange("p (a b) -> p a b", a=4),
                    in1=C["c_cm"][0:ncv, tt:tt + 1, :].broadcast_to([ncv, 4, 128]), op=ALU.mult),
                    reads=["ec%d" % eb, "c_cm"], writes=["ec%d" % eb])

                def pvc(e, eb=eb, ncv=ncv, h=h):
                    last = None
                    for g in range(4):
                        last = e.matmul(ps[3 + g][:, 0:161], lhsT=ec[eb][0:ncv, g * 128:(g + 1) * 128], rhs=self.vcc[0:ncv, h, :], start=True, stop=True)
                    return last
                S.add("pe", pvc, reads=["ec%d" % eb, "vcc"], writes=["ps3", "ps4", "ps5", "ps6"])
                for g in range(4):
                    S.add("dve", lambda e, g=g: e.tensor_scalar(out=rc[:, g:g + 1], in0=ps[3 + g][:, 128:129], scalar1=1e-30, scalar2=None, op0=ALU.max),
                          reads=["ps%d" % (3 + g)], writes=["nrc"])
                S.add("dve", lambda e: e.reciprocal(out=rc[:, 0:4], in_=rc[:, 0:4]), reads=["nrc"], writes=["nrc"])
                S.add("dve", lambda e, tt=tt, h=h: e.tensor_tensor(out=coef[:, 0:4], in0=rc[:, 0:4], in1=self.gsig[:, tt, 12 * h + 0:12 * h + 12:3], op=ALU.mult),
                      reads=["nrc", "gsig"], writes=["ncoef"])
                first_acc = True
                if 0 in self.nsa_br:
                    first_acc = False
                    for g in range(4):
                        S.add("dve", lambda e, g=g, h=h, oa=oa: e.tensor_scalar(out=oa[:, 4 * h + g, :], in0=ps[3 + g][:, 0:128],
                                                                                 scalar1=coef[:, g:g + 1], scalar2=None, op0=ALU.mult),
                              reads=["ps%d" % (3 + g), "ncoef"], writes=[oan])
                use_sel = tt >= 8
                sb_ = None
                if use_sel:
                    for g in range(4):
                        S.add("dve", lambda e, g=g: e.tensor_scalar(out=impg[:, g, :], in0=ps[3 + g][:, 129:161], scalar1=rc[:, g:g + 1], scalar2=None, op0=ALU.mult),
                              reads=["ps%d" % (3 + g), "nrc"], writes=["nimpg"])
                    S.add("dve", lambda e: e.tensor_reduce(out=imp[:], in_=impg[:].rearrange("p g j -> p j g"), axis=AX.X, op=ALU.add),
                          reads=["nimpg"], writes=["nimp"])
                    S.add("dve", lambda e, tt=tt: e.tensor_tensor(out=imp[:], in0=imp[:], in1=C["c_m1"][:, tt, :], op=ALU.mult), reads=["nimp", "c_m1"], writes=["nimp"])
                    S.add("dve", lambda e, tt=tt: e.tensor_tensor(out=imp[:], in0=imp[:], in1=C["c_m2"][:, tt, :], op=ALU.add), reads=["nimp", "c_m2"], writes=["nimp"])
                    S.add("dve", lambda e: e.tensor_tensor(out=cmpm[:], in0=imp[:].unsqueeze(1).broadcast_to([128, 32, 32]),
                                                           in1=imp[:].unsqueeze(2).broadcast_to([128, 32, 32]), op=ALU.is_gt),
                          reads=["nimp"], writes=["ncmpm"])
                    S.add("dve", lambda e: e.tensor_reduce(out=rk[:], in_=cmpm[:], axis=AX.X, op=ALU.add), reads=["ncmpm"], writes=["nrk"])
                    S.add("dve", lambda e: e.tensor_scalar(out=nsel[:], in0=rk[:], scalar1=15.5, scalar2=NEG, op0=ALU.is_gt, op1=ALU.mult),
                          reads=["nrk"], writes=["nsel"])
                    sb_ = nsl % 2
                    nsl += 1
                    ps7b = ps[7][:].bitcast(BF16)
                    S.add("pe", lambda e, ps7b=ps7b: e.transpose(out=ps7b[0:32, 0:128], in_=nsel[:], identity=C["c_ident"][:]),
                          reads=["nsel", "c_ident"], writes=["ps7"])
                    S.add("act", lambda e, sb_=sb_, ps7b=ps7b: e.copy(out=nselT[sb_][:], in_=ps7b[0:32, 0:128]), reads=["ps7"], writes=["nselT%d" % sb_])
                for br in (1, 2):
                    kT = self.ksT if br == 1 else self.kwT
                    kn = "ksT" if br == 1 else "kwT"
                    vv = self.vs if br == 1 else self.vw
                    vn = "vs" if br == 1 else "vw"
                    obank = 4 if br == 1 else 5
                    dcol = 132 if br == 1 else 136
                    dname = "ps6s" if br == 1 else "ps6w"
                    kts = list(range(0, tt + 1)) if br == 1 else list(range(max(0, tt - 4), tt + 1))
                    for ki, kt in enumerate(kts):
                        pb = nst % 3
                        nst += 1
                        extra = []
                        rds = [kn, "qT"]
                        if br == 1 and use_sel:
                            rds += ["c_E", "nselT%d" % sb_]
                        if kt == tt:
                            rds += ["c_ident", "c_cb"]
                        if br == 2 and kt == tt - 4:
                            rds += ["c_ident", "c_fb"]

                        def sc(e, pb=pb, kT=kT, h=h, kt=kt, Q=Q, br=br, sb_=sb_, tt=tt):
                            mms = [(ps[pb][:], kT[:, h, kt * 128:(kt + 1) * 128], Q)]
                            if br == 1 and tt >= 8:
                                for g in range(4):
                                    mms.append((ps[pb][:, g * 128:(g + 1) * 128], C["c_E"][:, kt, :], nselT[sb_][:]))
                            if kt == tt:
                                mms.append((ps[pb][:], C["c_ident"][:], C["c_cb"][:]))
                            if br == 2 and kt == tt - 4:
                                mms.append((ps[pb][:], C["c_ident"][:], C["c_fb"][:]))
                            last = None
                            for i, (o, l, r) in enumerate(mms):
                                last = e.matmul(o, lhsT=l, rhs=r, start=(i == 0), stop=(i == len(mms) - 1))
                            return last
                        S.add("pe", sc, reads=rds, writes=["ps%d" % pb])
                        pi = npt % 3
                        npt += 1
                        S.add("act", lambda e, pi=pi, pb=pb: e.activation(out=pT[pi][:], in_=ps[pb][:], func=AF.Exp, scale=SCALE),
                              reads=["ps%d" % pb], writes=["npT%d" % pi])

                        def pv(e, pi=pi, vv=vv, kt=kt, h=h, first=(ki == 0), lastk=(ki == len(kts) - 1)):
                            last = None
                            for g in range(4):
                                last = e.matmul(ps[3 + g][:, 0:129], lhsT=pT[pi][:, g * 128:(g + 1) * 128],
                                                rhs=vv[:, kt, h, :], start=first, stop=lastk)
                            return last
                        S.add("pe", pv, reads=["npT%d" % pi, vn], writes=["ps3", "ps4", "ps5", "ps6"])
                    for g in range(4):
                        S.add("dve", lambda e, g=g: e.tensor_copy(out=rc[:, g:g + 1], in_=ps[3 + g][:, 128:129]), reads=["ps%d" % (3 + g)], writes=["nrc"])
                    S.add("dve", lambda e: e.reciprocal(out=rc[:, 0:4], in_=rc[:, 0:4]), reads=["nrc"], writes=["nrc"])
                    S.add("dve", lambda e, tt=tt, h=h, br=br: e.tensor_tensor(out=coef[:, 0:4], in0=rc[:, 0:4], in1=self.gsig[:, tt, 12 * h + br:12 * h + 12:3], op=ALU.mult),
                          reads=["nrc", "gsig"], writes=["ncoef"])
                    if br not in self.nsa_br:
                        continue
                    for g in range(4):
                        if first_acc:
                            S.add("dve", lambda e, g=g, h=h, oa=oa: e.tensor_scalar(out=oa[:, 4 * h + g, :], in0=ps[3 + g][:, 0:128],
                                                                                      scalar1=coef[:, g:g + 1], scalar2=None, op0=ALU.mult),
                                  reads=["ps%d" % (3 + g), "ncoef"], writes=[oan])
                        else:
                            S.add("dve", lambda e, g=g, h=h, oa=oa: e.scalar_tensor_tensor(
                                out=oa[:, 4 * h + g, :], in0=ps[3 + g][:, 0:128], scalar=coef[:, g:g + 1], in1=oa[:, 4 * h + g, :],
                                op0=ALU.mult, op1=ALU.add),
                                reads=["ps%d" % (3 + g), "ncoef", oan], writes=[oan])
                    first_acc = False
            b = tt % 2
            self.rmsnorm_store(oa[:].rearrange("p a b -> p (a b)"), 1024, gnn[:], "bc_gnn", ob[b][:], "nob%d" % b,
                               self.mixed[t0:t0 + 128, 0:1024], tmp, oan)
        S.emit()

    def layernorm(self, src, skey, dst, dkey, g_t, gname, b_t, bname, tmp):
        S = self.S
        stats, mv, sq, rstd = tmp
        for c in range(4):
            S.add("dve", lambda e, c=c: e.bn_stats(out=stats[:, c, :], in_=src[:, c * 512:(c + 1) * 512]), reads=[skey], writes=["ln_stats"])
        S.add("dve", lambda e: e.bn_aggr(out=mv[:, 0:2], in_=stats[:].rearrange("p a b -> p (a b)")), reads=["ln_stats"], writes=["ln_mv"])
        S.add("act", lambda e: e.activation(out=sq[:, 0:1], in_=mv[:, 1:2], func=AF.Sqrt, bias=EPS), reads=["ln_mv"], writes=["ln_sq"])
        S.add("dve", lambda e: e.reciprocal(out=rstd[:, 0:1], in_=sq[:, 0:1]), reads=["ln_sq"], writes=["ln_rstd"])
        S.add("dve", lambda e: e.tensor_scalar(out=dst, in0=src, scalar1=mv[:, 0:1], scalar2=rstd[:, 0:1], op0=ALU.subtract, op1=ALU.mult),
              reads=[skey, "ln_mv", "ln_rstd"], writes=[dkey])
        S.add("pool", lambda e: e.tensor_tensor(out=dst, in0=dst, in1=g_t[:], op=ALU.mult), reads=[dkey, gname], writes=[dkey])
        S.add("pool", lambda e: e.tensor_tensor(out=dst, in0=dst, in1=b_t[:], op=ALU.add), reads=[dkey, bname], writes=[dkey])

    def ln_tmp(self, st, tag):
        return (self.sb(st, "lns" + tag, [128, 4, 6], F32), self.sb(st, "lnm" + tag, [128, 2], F32),
                self.sb(st, "lnq" + tag, [128, 1], F32), self.sb(st, "lnr" + tag, [128, 1], F32))

    def phase_wout(self, st):
        S, I, C = self.S, self.I, self.C
        ps = self.ps
        P_ = self._persist
        self.slots = self.sb(P_, "slots", [128, 16, 4], I32)
        self.gates = self.sb(P_, "gatesk", [128, 16, 4], F32)
        self.cnti = self.sb(P_, "cnti", [128, 32], I32)
        S.cnt_ap = self.cnti
        wo = self.sb(st, "wo", [128, 16, 2048], BF16)
        for cg in range(4):
            self.dma("pool", wo[:, :, cg * 512:(cg + 1) * 512], I["w_out"][:, cg * 512:(cg + 1) * 512].rearrange("(k p) n -> p k n", p=128), writes=["wo"])
        wr = self.sb(st, "wr", [128, 16, 32], F32)
        self.dma("sp", wr[:], I["w_router"].rearrange("(k p) n -> p k n", p=128), writes=["wr"])
        g1 = self.bcast_tile(st, "g1", I["ln1_g"], 2048)
        b1 = self.bcast_tile(st, "b1", I["ln1_b"], 2048)
        br = self.bcast_tile(st, "br", I["b_router"], 32)
        mx = [self.sb(st, "mx%d" % i, [128, 2048], BF16) for i in range(2)]
        mxT = [self.sb(st, "mxT%d" % i, [128, 16, 128], BF16) for i in range(2)]
        xt = [self.sb(st, "xres%d" % i, [128, 2048], F32) for i in range(2)]
        pre = self.sb(st, "pre", [128, 2048], F32)
        hh = [self.sb(st, "hh%d" % i, [128, 2048], F32) for i in range(2)]
        hb = [self.sb(st, "hb%d" % i, [128, 2048], BF16) for i in range(2)]
        hT = self.sb(st, "hT", [128, 16, 128], F32)
        lg = self.sb(st, "lg", [128, 32], F32)
        cm = self.sb(st, "rcm", [128, 32, 32], F32)
        rk = self.sb(st, "rrk", [128, 32], F32)
        sel4 = self.sb(st, "sel4", [128, 32], F32)
        selb = self.sb(st, "selb", [128, 32], BF16)
        mxv = self.sb(st, "rmx", [128, 1], F32)
        ex = self.sb(st, "rex", [128, 32], F32)
        dn = self.sb(st, "rdn", [128, 1], F32)
        carry = self.sb(st, "carry", [128, 32], F32)
        addr = self.sb(st, "addr", [128, 32], F32)
        oh = self.sb(st, "oh", [128, 32], F32)
        t32 = self.sb(st, "t32", [128, 32], F32)
        slf = self.sb(st, "slf", [128, 4], F32)
        lt = self.ln_tmp(st, "1")
        S.add("dve", lambda e: e.memset(carry[:], 0.0), writes=["carry"])
        for tt in range(16):
            b = tt % 2
            t0 = tt * 128
            self.dma("sp", mx[b][:], self.mixed[t0:t0 + 128, :], reads=["mixed"], writes=["mx%d" % b])
            self.dma("act", xt[b][:], I["x"][t0:t0 + 128, :], writes=["xres%d" % b])
            for grp in range(4):
                psv = ps[grp % 2][:].bitcast(BF16)

                def tr(e, b=b, grp=grp, psv=psv):
                    last = None
                    for i in range(4):
                        dk = grp * 4 + i
                        last = e.transpose(out=psv[:, i * 128:(i + 1) * 128], in_=mx[b][:, dk * 128:(dk + 1) * 128], identity=C["c_ident"][:])
                    return last
                S.add("pe", tr, reads=["mx%d" % b, "c_ident"], writes=["ps%d" % (grp % 2)])
                dst = mxT[b][:, grp * 4:(grp + 1) * 4, :]
                src = psv[:, 0:512].rearrange("p (a b) -> p a b", a=4)
                S.add("act", lambda e, dst=dst, src=src: e.copy(out=dst, in_=src), reads=["ps%d" % (grp % 2)], writes=["mxT%d" % b])
            for cg in range(4):
                pb = 2 + cg % 2
                pairs = [(mxT[b][:, fk, :], wo[:, fk, cg * 512:(cg + 1) * 512]) for fk in range(16)]
                self.mm_group(ps[pb][:], pairs, reads=["mxT%d" % b, "wo"], writes=["ps%d" % pb])
                S.add("dve", lambda e, cg=cg, pb=pb, b=b: e.scalar_tensor_tensor(out=pre[:, cg * 512:(cg + 1) * 512], in0=xt[b][:, cg * 512:(cg + 1) * 512],
                                                                               scalar=ALPHA, in1=ps[pb][:], op0=ALU.mult, op1=ALU.add),
                      reads=["ps%d" % pb, "xres%d" % b], writes=["pre"])
            self.layernorm(pre[:], "pre", hh[b][:], "hh%d" % b, g1, "bc_g1", b1, "bc_b1", lt)
            self.dma("sp", self.hf[t0:t0 + 128, :], hh[b][:], reads=["hh%d" % b], writes=["hf"])
            S.add("act", lambda e, b=b: e.copy(out=hb[b][:], in_=hh[b][:]), reads=["hh%d" % b], writes=["hb%d" % b])
            for grp in range(4):
                pb = 4 + grp % 2

                def trf(e, b=b, grp=grp, pb=pb):
                    last = None
                    for i in range(4):
                        dk = grp * 4 + i
                        last = e.transpose(out=ps[pb][:, i * 128:(i + 1) * 128], in_=hh[b][:, dk * 128:(dk + 1) * 128], identity=C["ident_f"][:])
                    return last
                S.add("pe", trf, reads=["hh%d" % b, "ident_f"], writes=["ps%d" % pb])
                S.add("act", lambda e, grp=grp, pb=pb: e.copy(out=hT[:, grp * 4:(grp + 1) * 4, :], in_=ps[pb][:].rearrange("p (a b) -> p a b", a=4)),
                      reads=["ps%d" % pb], writes=["hT"])
            pairs = [(hT[:, dk, :], wr[:, dk, :]) for dk in range(16)]
            self.mm_group(ps[6][:, 0:32], pairs, reads=["hT", "wr"], writes=["ps6"])
            S.add("dve", lambda e: e.tensor_tensor(out=lg[:], in0=ps[6][:, 0:32], in1=br[:], op=ALU.add), reads=["ps6", "bc_br"], writes=["lg"])
            if self.dbg == "logits":
                self.dma("sp", self.dlog[t0:t0 + 128, 0:32], lg[:], reads=["lg"], writes=["dlog"])
            S.add("dve", lambda e: e.tensor_tensor(out=cm[:], in0=lg[:].unsqueeze(1).broadcast_to([128, 32, 32]),
                                                   in1=lg[:].unsqueeze(2).broadcast_to([128, 32, 32]), op=ALU.is_gt), reads=["lg"], writes=["rcm"])
            S.add("dve", lambda e: e.tensor_reduce(out=rk[:], in_=cm[:], axis=AX.X, op=ALU.add), reads=["rcm"], writes=["rrk"])
            S.add("dve", lambda e: e.tensor_scalar(out=sel4[:], in0=rk[:], scalar1=3.5, scalar2=None, op0=ALU.is_lt), reads=["rrk"], writes=["sel4"])
            S.add("dve", lambda e: e.tensor_copy(out=selb[:], in_=sel4[:]), reads=["sel4"], writes=["selb"])
            S.add("dve", lambda e: e.tensor_reduce(out=mxv[:], in_=lg[:], axis=AX.X, op=ALU.max), reads=["lg"], writes=["rmx"])
            S.add("dve", lambda e: e.tensor_scalar(out=ex[:], in0=lg[:], scalar1=mxv[:, 0:1], scalar2=None, op0=ALU.subtract), reads=["lg", "rmx"], writes=["rex"])
            S.add("act", lambda e: e.activation(out=ex[:], in_=ex[:], func=AF.Exp), reads=["rex"], writes=["rex"])
            S.add("dve", lambda e: e.tensor_tensor(out=ex[:], in0=ex[:], in1=sel4[:], op=ALU.mult), reads=["rex", "sel4"], writes=["rex"])
            S.add("dve", lambda e: e.tensor_reduce(out=dn[:], in_=ex[:], axis=AX.X, op=ALU.add), reads=["rex"], writes=["rdn"])
            S.add("dve", lambda e: e.reciprocal(out=dn[:], in_=dn[:]), reads=["rdn"], writes=["rdn"])
            S.add("dve", lambda e: e.tensor_scalar(out=ex[:], in0=ex[:], scalar1=dn[:, 0:1], scalar2=None, op0=ALU.mult), reads=["rex", "rdn"], writes=["rex"])
            self.mm_group(ps[7][:, 0:32], [(C["c_us"][:], selb[:])], reads=["c_us", "selb"], writes=["ps7a"])
            self.mm_group(ps[7][:, 32:64], [(C["c_ones"][:], selb[:])], reads=["c_ones", "selb"], writes=["ps7b"])
            S.add("dve", lambda e: e.tensor_tensor(out=addr[:], in0=ps[7][:, 0:32], in1=carry[:], op=ALU.add), reads=["ps7a", "carry"], writes=["addr"])
            S.add("dve", lambda e: e.tensor_tensor(out=carry[:], in0=ps[7][:, 32:64], in1=carry[:], op=ALU.add), reads=["ps7b", "carry"], writes=["carry"])
            S.add("dve", lambda e: e.tensor_scalar(out=addr[:], in0=addr[:], scalar1=float(CAP - 1), scalar2=None, op0=ALU.min), reads=["addr"], writes=["addr"])
            S.add("dve", lambda e: e.tensor_tensor(out=addr[:], in0=addr[:], in1=C["c_ecap"][:], op=ALU.add), reads=["addr", "c_ecap"], writes=["addr"])
            for k in range(4):
                S.add("dve", lambda e, k=k: e.tensor_scalar(out=oh[:], in0=rk[:], scalar1=float(k), scalar2=None, op0=ALU.is_equal), reads=["rrk"], writes=["oh"])
                S.add("dve", lambda e: e.tensor_tensor(out=t32[:], in0=oh[:], in1=addr[:], op=ALU.mult), reads=["oh", "addr"], writes=["t32"])
                S.add("dve", lambda e, k=k: e.tensor_reduce(out=slf[:, k:k + 1], in_=t32[:], axis=AX.X, op=ALU.add), reads=["t32"], writes=["slf"])
                S.add("dve", lambda e: e.tensor_tensor(out=t32[:], in0=oh[:], in1=ex[:], op=ALU.mult), reads=["oh", "rex"], writes=["t32"])
                S.add("dve", lambda e, k=k, tt=tt: e.tensor_reduce(out=self.gates[:, tt, k:k + 1], in_=t32[:], axis=AX.X, op=ALU.add), reads=["t32"], writes=["gatesk"])
            S.add("dve", lambda e, tt=tt: e.tensor_copy(out=self.slots[:, tt, :], in_=slf[:]), reads=["slf"], writes=["slots"])
            if self.dbg == "logits":
                self.dma("sp", self.dlog[t0:t0 + 128, 32:36], slf[:], reads=["slf"], writes=["dlog"])
                self.dma("sp", self.dlog[t0:t0 + 128, 36:40], self.gates[:, tt, :], reads=["gatesk"], writes=["dlog"])
            for k in range(4):
                S.add("pool", lambda e, k=k, tt=tt, b=b: e.indirect_dma_start(
                    out=self.xbuf, out_offset=bass.IndirectOffsetOnAxis(ap=self.slots[:, tt, k:k + 1], axis=0),
                    in_=hb[b][:], in_offset=None),
                    reads=["hb%d" % b, "slots"], writes=["xbuf"], dma=True)
        S.add("dve", lambda e: e.tensor_copy(out=self.cnti[:], in_=carry[:]), reads=["carry"], writes=["cnti"])
        S.emit()

    def phase_experts(self, st):
        S, I, C = self.S, self.I, self.C
        ps = self.ps
        NW = 3
        NS = CAP // 128
        CW = 256
        NCH = CAP // CW
        wsl = [self.sb(st, "ew%d" % i, [128, 16, 512], BF16) for i in range(NW)]
        xin = [self.sb(st, "exin%d" % i, [128, 2048], BF16) for i in range(2)]
        XT = self.sb(st, "eXT", [128, 16, CAP], BF16)
        actT = self.sb(st, "eact", [128, 16, CAP], BF16)
        ysm = [self.sb(st, "eyo%d" % i, [128, 512], BF16) for i in range(4)]
        bgu = [self.sb(st, "ebgu%d" % i, [1, 4096], BF16) for i in range(2)]
        bdn = [self.sb(st, "ebdn%d" % i, [1, 2048], BF16) for i in range(2)]
        onesr = self.sb(st, "eones", [1, 512], BF16)
        xg = [self.sb(st, "exg%d" % i, [128, 512], F32) for i in range(2)]
        sg = [self.sb(st, "esg%d" % i, [128, 512], F32) for i in range(2)]
        tl = [self.sb(st, "etl%d" % i, [128, 512], F32) for i in range(2)]
        S.add("dve", lambda e: e.memset(onesr[:], 1.0), writes=["eones"])

        def skipper(bank, col0=0):
            return lambda e: e.matmul(ps[bank][:, col0:col0 + 2], lhsT=onesr[0:1, 0:128], rhs=onesr[0:1, 0:2], start=True, stop=True)
        S.add("pool", lambda e: e.memset(XT[:], 0.0), writes=["eXT"])
        nw = 0
        nf = 0
        ny = 0
        ntr = 0
        nx = 0
        for ex in range(NEXP):
            b = ex % 2
            self.dma("pool", bgu[b][:], I["b_gu"][ex:ex + 1, :], writes=["ebgu%d" % b])
            self.dma("pool", bdn[b][:], I["b_dn"][ex:ex + 1, :], writes=["ebdn%d" % b])
            for c in range(NS):
                pr = (ex, c * 128)
                xb = nx % 2
                nx += 1
                self.dma("sp", xin[xb][:], self.xbuf[ex * CAP + c * 128:ex * CAP + (c + 1) * 128, :], reads=["xbuf"], writes=["exin%d" % xb])
                for grp in range(4):
                    pb = ntr % 2
                    ntr += 1
                    psv = ps[pb][:].bitcast(BF16)

                    def tr(e, xb=xb, grp=grp, psv=psv):
                        last = None
                        for i in range(4):
                            dk = grp * 4 + i
                            last = e.transpose(out=psv[:, i * 128:(i + 1) * 128], in_=xin[xb][:, dk * 128:(dk + 1) * 128], identity=C["c_ident"][:])
                        return last
                    S.add("pe", tr, reads=["exin%d" % xb, "c_ident", "eones"], writes=["ps%d" % pb], pred=(pr[0], pr[1], skipper(pb)))
                    dst = XT[:, grp * 4:(grp + 1) * 4, c * 128:(c + 1) * 128]
                    src = psv[:, 0:512].rearrange("p (a b) -> p a b", a=4)
                    S.add("act", lambda e, dst=dst, src=src: e.copy(out=dst, in_=src), reads=["ps%d" % pb], writes=["eXT"])
            for s in range(8):
                wb = nw % NW
                nw += 1
                self.dma("pool", wsl[wb][:], I["w_gu"][ex][:, s * 512:(s + 1) * 512].rearrange("(k p) n -> p k n", p=128), writes=["ew%d" % wb])
                for ftl in range(2):
                    ft = 2 * s + ftl
                    for hb in range(NCH // 2):
                        fb = nf % 2
                        nf += 1
                        pg, pl = 2 + 2 * fb, 3 + 2 * fb
                        for sub in range(2):
                            ch = 2 * hb + sub
                            pr = (ex, ch * CW)
                            for which, pbk in ((0, pg), (1, pl)):
                                pairs = [(wsl[wb][:, dk, ftl * 256 + which:ftl * 256 + 256:2], XT[:, dk, ch * CW:(ch + 1) * CW]) for dk in range(16)]
                                pairs.append((bgu[b][0:1, ft * 256 + which:ft * 256 + 256:2], onesr[0:1, 0:CW]))
                                self.mm_group(ps[pbk][:, sub * CW:(sub + 1) * CW], pairs, reads=["ew%d" % wb, "eXT", "ebgu%d" % b, "eones"], writes=["ps%d" % pbk],
                                              pred=(pr[0], pr[1], skipper(pbk, sub * CW)))
                        S.add("dve", lambda e, fb=fb, pg=pg: e.tensor_scalar(out=xg[fb][:], in0=ps[pg][:], scalar1=7.0, scalar2=None, op0=ALU.min),
                              reads=["ps%d" % pg], writes=["exg%d" % fb])
                        S.add("act", lambda e, fb=fb: e.activation(out=sg[fb][:], in_=xg[fb][:], func=AF.Sigmoid, scale=1.702),
                              reads=["exg%d" % fb], writes=["esg%d" % fb])
                        S.add("dve", lambda e, fb=fb, pl=pl: e.tensor_scalar(out=tl[fb][:], in0=ps[pl][:], scalar1=1.0, scalar2=-6.0, op0=ALU.add, op1=ALU.max),
                              reads=["ps%d" % pl], writes=["etl%d" % fb])
                        S.add("dve", lambda e, fb=fb: e.scalar_tensor_tensor(out=tl[fb][:], in0=tl[fb][:], scalar=8.0, in1=xg[fb][:], op0=ALU.min, op1=ALU.mult),
                              reads=["etl%d" % fb, "exg%d" % fb], writes=["etl%d" % fb])
                        S.add("dve", lambda e, fb=fb, ft=ft, hb=hb: e.tensor_tensor(out=actT[:, ft, hb * 512:(hb + 1) * 512], in0=tl[fb][:], in1=sg[fb][:], op=ALU.mult),
                              reads=["etl%d" % fb, "esg%d" % fb], writes=["eact"])
            for cg in range(4):
                wb = nw % NW
                nw += 1
                self.dma("pool", wsl[wb][:], I["w_dn"][ex][:, cg * 512:(cg + 1) * 512].rearrange("(k p) n -> p k n", p=128), writes=["ew%d" % wb])
                for c in range(NS):
                    pr = (ex, c * 128)
                    pb = 6 + ny % 2
                    yb = ny % 4
                    ny += 1
                    pairs = [(actT[:, fk, c * 128:(c + 1) * 128], wsl[wb][:, fk, :]) for fk in range(16)]
                    pairs.append((onesr[0:1, 0:128], bdn[b][0:1, cg * 512:(cg + 1) * 512]))
                    self.mm_group(ps[pb][:], pairs, reads=["ew%d" % wb, "eact", "ebdn%d" % b, "eones"], writes=["ps%d" % pb], pred=(pr[0], pr[1], skipper(pb)))
                    S.add("act", lambda e, pb=pb, yb=yb: e.copy(out=ysm[yb][:], in_=ps[pb][:]), reads=["ps%d" % pb], writes=["eyo%d" % yb])
                    r0 = ex * CAP + c * 128
                    self.dma("sp", self.ysb[r0:r0 + 128, cg * 512:(cg + 1) * 512], ysm[yb][:], reads=["eyo%d" % yb])
        S.emit()

    def phase_combine(self, st):
        S, I, C = self.S, self.I, self.C
        g2 = self.bcast_tile(st, "g2", I["ln2_g"], 2048)
        b2 = self.bcast_tile(st, "b2", I["ln2_b"], 2048)
        yk = [self.sb(st, "yk%d" % i, [128, 4, 2048], BF16) for i in range(2)]
        ht = [self.sb(st, "cht%d" % i, [128, 2048], F32) for i in range(2)]
        acc = self.sb(st, "cacc", [128, 2048], F32)
        ot = [self.sb(st, "cot%d" % i, [128, 2048], F32) for i in range(2)]
        lt = self.ln_tmp(st, "2")
        for tt in range(16):
            b = tt % 2
            t0 = tt * 128
            for k in range(4):
                S.add("pool", lambda e, k=k, tt=tt, b=b: e.indirect_dma_start(
                    out=yk[b][:, k, :], out_offset=None, in_=self.ysb,
                    in_offset=bass.IndirectOffsetOnAxis(ap=self.slots[:, tt, k:k + 1], axis=0)),
                    reads=["ysb", "slots"], writes=["yk%d_%d" % (b, k)], dma=True)
            self.dma("sp", ht[b][:], self.hf[t0:t0 + 128, :], reads=["hf"], writes=["cht%d" % b])
            S.add("act", lambda e, b=b: e.mul(out=acc[:], in_=ht[b][:], mul=ALPHA), reads=["cht%d" % b], writes=["cacc"])
            for k in range(4):
                S.add("dve", lambda e, k=k, tt=tt, b=b: e.scalar_tensor_tensor(out=acc[:], in0=yk[b][:, k, :], scalar=self.gates[:, tt, k:k + 1], in1=acc[:],
                                                                            op0=ALU.mult, op1=ALU.add),
                      reads=["yk%d_%d" % (b, k), "gatesk", "cacc"], writes=["cacc"])
            self.layernorm(acc[:], "cacc", ot[b][:], "cot%d" % b, g2, "bc_g2", b2, "bc_b2", lt)
            self.dma("sp", self.out[t0:t0 + 128, :], ot[b][:], reads=["cot%d" % b], writes=["out"])
        S.emit()


_CACHE = {}


def _get_nc(dbg=None):
    if dbg not in _CACHE:
        k = K(dbg)
        _CACHE[dbg] = k.build()
    return _CACHE[dbg]


def kernel(dbg=None, **inputs):
    consts = make_consts()
    nc = _get_nc(dbg)
    shared = {}
    for n, shp in IN_SHAPES.items():
        if n in ("x", "mem") or (dbg is not None and n in BIGW):
            continue
        a = np.asarray(inputs[n])
        shared[n] = np.ascontiguousarray(a.reshape(shp).astype(np.float32, copy=False))
    shared.update(consts)
    x = np.asarray(inputs["x"])
    mem = np.asarray(inputs["mem"])
    in_maps = []
    for b in range(8):
        m = dict(shared)
        m["x"] = np.ascontiguousarray(x[b])
        m["mem"] = np.ascontiguousarray(mem[b])
        in_maps.append(m)
    res = run_bass_kernel_spmd(nc, in_maps, core_ids=list(range(8)))
    if dbg is not None:
        return res.results
    return np.stack([np.asarray(r["out"]) for r in res.results], axis=0).astype(np.float32, copy=False)
```
